# Optimizing a Trainium2 kernel written in Bass

```python
import math
import jax, jax.numpy as jnp
from jax import lax
import numpy as np

D_MODEL = 1024
BATCH = 8
SEQ = 2048
DEPTH = 1

D_MIX = D_MODEL
D_ATTN = D_MIX // 2
HEAD_DIM = 64
N_HEADS = D_ATTN // HEAD_DIM
N_KV_HEADS = 2
Q_PER_KV = N_HEADS // N_KV_HEADS
D_KV = N_KV_HEADS * HEAD_DIM
WINDOW = 128
BLOCK = 128
D_SSM = D_MIX - D_ATTN
SSM_GROUP = 16
N_SSM_GROUPS = D_SSM // SSM_GROUP
STATE = 64
D_IN = D_ATTN + 2 * D_KV + D_SSM
N_EXPERTS = 32
TOP_K = 4
D_FF = D_MODEL
SWIGLU_LIMIT = 7.0
SWIGLU_ALPHA = 1.702
EPS = 1e-6
NEG_INF = -1e30

kernel_name = "hybrid_swa_s5_moe_adaln_block"


def rmsnorm(x, gain):
    x32 = x.astype(jnp.float32)
    y = x32 * lax.rsqrt(jnp.mean(x32 * x32, axis=-1, keepdims=True) + EPS)
    return (y * gain.astype(jnp.float32)).astype(x.dtype)


def sliding_window_attention(q, k, v, sinks):
    B, S = q.shape[0], q.shape[1]
    nb = S // BLOCK
    qb = q.reshape(B, nb, BLOCK, N_KV_HEADS, Q_PER_KV, HEAD_DIM)
    pad = ((0, 0), (BLOCK, 0), (0, 0), (0, 0))
    kp = jnp.pad(k, pad).reshape(B, nb + 1, BLOCK, N_KV_HEADS, HEAD_DIM)
    vp = jnp.pad(v, pad).reshape(B, nb + 1, BLOCK, N_KV_HEADS, HEAD_DIM)
    kw = jnp.concatenate([kp[:, :-1], kp[:, 1:]], axis=2)
    vw = jnp.concatenate([vp[:, :-1], vp[:, 1:]], axis=2)
    scores = jnp.einsum('bnqhgd,bnkhd->bnhgqk', qb, kw).astype(jnp.float32)
    scores = scores * (1.0 / math.sqrt(HEAD_DIM))
    qi = jnp.arange(BLOCK)[:, None]
    kj = jnp.arange(2 * BLOCK)[None, :]
    diff = BLOCK + qi - kj
    band = (diff >= 0) & (diff < WINDOW)
    kpos = jnp.arange(nb)[:, None, None] * BLOCK - BLOCK + kj[None]
    mask = band[None] & (kpos >= 0)
    scores = jnp.where(mask[None, :, None, None], scores, NEG_INF)
    sink = sinks.astype(jnp.float32).reshape(N_KV_HEADS, Q_PER_KV)
    sink = jnp.broadcast_to(sink[None, None, :, :, None, None],
                            scores.shape[:-1] + (1,))
    probs = jax.nn.softmax(jnp.concatenate([scores, sink], axis=-1), axis=-1)[..., :-1]
    out = jnp.einsum('bnhgqk,bnkhd->bnqhgd', probs.astype(v.dtype), vw)
    return out.reshape(B, S, D_ATTN)


def _complex_affine_combine(e1, e2):
    a1r, a1i, b1r, b1i = e1
    a2r, a2i, b2r, b2i = e2
    ar = a2r * a1r - a2i * a1i
    ai = a2r * a1i + a2i * a1r
    br = a2r * b1r - a2i * b1i + b2r
    bi = a2r * b1i + a2i * b1r + b2i
    return (ar, ai, br, bi)


def s5_ssm(u, lam_re, lam_im, log_dt, b_re, b_im, c_re, c_im, d_skip):
    B, S = u.shape[0], u.shape[1]
    ug = u.astype(jnp.float32).reshape(B, S, N_SSM_GROUPS, SSM_GROUP)
    lr = lam_re.astype(jnp.float32)
    li = lam_im.astype(jnp.float32)
    dt = jnp.exp(log_dt.astype(jnp.float32))[:, None]
    mag = jnp.exp(lr * dt)
    lb_r = mag * jnp.cos(li * dt)
    lb_i = mag * jnp.sin(li * dt)
    den = lr * lr + li * li
    coef_r = ((lb_r - 1.0) * lr + lb_i * li) / den
    coef_i = (lb_i * lr - (lb_r - 1.0) * li) / den
    br = b_re.astype(jnp.float32)
    bi = b_im.astype(jnp.float32)
    bbar_r = coef_r[..., None] * br - coef_i[..., None] * bi
    bbar_i = coef_r[..., None] * bi + coef_i[..., None] * br
    bu_r = jnp.einsum('bsgh,gph->bsgp', ug, bbar_r)
    bu_i = jnp.einsum('bsgh,gph->bsgp', ug, bbar_i)
    a_r = jnp.broadcast_to(lb_r, bu_r.shape)
    a_i = jnp.broadcast_to(lb_i, bu_i.shape)
    _, _, x_r, x_i = lax.associative_scan(_complex_affine_combine,
                                          (a_r, a_i, bu_r, bu_i), axis=1)
    y = (jnp.einsum('bsgp,ghp->bsgh', x_r, c_re.astype(jnp.float32))
         - jnp.einsum('bsgp,ghp->bsgh', x_i, c_im.astype(jnp.float32)))
    y = y + d_skip.astype(jnp.float32).reshape(N_SSM_GROUPS, SSM_GROUP) * ug
    return y.reshape(B, S, D_SSM).astype(u.dtype)


def moe_ffn(h, w_router, b_router, w_gate_up, b_gate_up, w_down, b_down):
    B, S, D = h.shape
    T = B * S
    ht = h.reshape(T, D)
    logits = (ht @ w_router + b_router).astype(jnp.float32)
    top_val, top_idx = lax.top_k(logits, TOP_K)
    weights = jax.nn.softmax(top_val, axis=-1)
    flat_e = top_idx.reshape(-1)
    order = jnp.argsort(flat_e)
    tok = order // TOP_K
    e_sorted = flat_e[order]
    sizes = jnp.bincount(flat_e, length=N_EXPERTS).astype(jnp.int32)
    xs = ht[tok]
    gu = lax.ragged_dot(xs, w_gate_up, sizes) + b_gate_up[e_sorted]
    gate = jnp.minimum(gu[:, ::2], SWIGLU_LIMIT)
    up = jnp.clip(gu[:, 1::2], -SWIGLU_LIMIT, SWIGLU_LIMIT)
    act = (up + 1.0) * (gate * jax.nn.sigmoid(SWIGLU_ALPHA * gate))
    ys = lax.ragged_dot(act, w_down, sizes) + b_down[e_sorted]
    ys = ys * weights.reshape(-1)[order][:, None].astype(ys.dtype)
    out = jnp.zeros((T, D), ys.dtype).at[tok].add(ys)
    return out.reshape(B, S, D)


def setup_inputs(seed: int = 0) -> dict:
    key = jax.random.key(seed)
    ks = jax.random.split(key, 32)
    L = DEPTH

    def nrm(k, shape, scale):
        return jax.random.normal(k, shape, jnp.float32) * scale

    def gain(k, shape):
        return 1.0 + nrm(k, shape, 0.02)

    lam_im0 = jnp.pi * jnp.arange(STATE, dtype=jnp.float32)
    return {
        "x": nrm(ks[0], (BATCH, SEQ, D_MODEL), 1.0),
        "c": nrm(ks[1], (BATCH, D_MODEL), 1.0),
        "w_ada": nrm(ks[2], (L, D_MODEL, 6 * D_MODEL), 0.5 * D_MODEL ** -0.5),
        "b_ada": nrm(ks[3], (L, 6 * D_MODEL), 0.02),
        "norm_mix": gain(ks[4], (L, D_MODEL)),
        "w_in": nrm(ks[5], (L, D_MODEL, D_IN), D_MODEL ** -0.5),
        "b_in": nrm(ks[6], (L, D_IN), 0.02),
        "q_norm": gain(ks[7], (L, HEAD_DIM)),
        "k_norm": gain(ks[8], (L, HEAD_DIM)),
        "sinks": nrm(ks[9], (L, N_HEADS), 0.5),
        "lam_re": -0.5 + nrm(ks[10], (L, N_SSM_GROUPS, STATE), 0.01),
        "lam_im": lam_im0 + nrm(ks[11], (L, N_SSM_GROUPS, STATE), 0.01),
        "log_dt": jax.random.uniform(ks[12], (L, N_SSM_GROUPS), jnp.float32,
                                     minval=math.log(1e-3), maxval=math.log(1e-1)),
        "b_re": nrm(ks[13], (L, N_SSM_GROUPS, STATE, SSM_GROUP), (2 * SSM_GROUP) ** -0.5),
        "b_im": nrm(ks[14], (L, N_SSM_GROUPS, STATE, SSM_GROUP), (2 * SSM_GROUP) ** -0.5),
        "c_re": nrm(ks[15], (L, N_SSM_GROUPS, SSM_GROUP, STATE), (2 * STATE) ** -0.5),
        "c_im": nrm(ks[16], (L, N_SSM_GROUPS, SSM_GROUP, STATE), (2 * STATE) ** -0.5),
        "d_skip": nrm(ks[17], (L, D_SSM), 1.0),
        "w_glu": nrm(ks[18], (L, D_SSM, D_SSM), D_SSM ** -0.5),
        "b_glu": nrm(ks[19], (L, D_SSM), 0.02),
        "attn_out_norm": gain(ks[20], (L, D_ATTN)),
        "ssm_out_norm": gain(ks[21], (L, D_SSM)),
        "w_out": nrm(ks[22], (L, D_MIX, D_MODEL), D_MIX ** -0.5),
        "norm_ffn": gain(ks[23], (L, D_MODEL)),
        "w_router": nrm(ks[24], (L, D_MODEL, N_EXPERTS), D_MODEL ** -0.5),
        "b_router": nrm(ks[25], (L, N_EXPERTS), 0.01),
        "w_gate_up": nrm(ks[26], (L, N_EXPERTS, D_MODEL, 2 * D_FF), D_MODEL ** -0.5),
        "b_gate_up": nrm(ks[27], (L, N_EXPERTS, 2 * D_FF), 0.02),
        "w_down": nrm(ks[28], (L, N_EXPERTS, D_FF, D_MODEL), D_FF ** -0.5),
        "b_down": nrm(ks[29], (L, N_EXPERTS, D_MODEL), 0.02),
    }


def reference(x, c, w_ada, b_ada, norm_mix, w_in, b_in, q_norm, k_norm, sinks,
              lam_re, lam_im, log_dt, b_re, b_im, c_re, c_im, d_skip, w_glu, b_glu,
              attn_out_norm, ssm_out_norm, w_out, norm_ffn, w_router, b_router,
              w_gate_up, b_gate_up, w_down, b_down):
    B, S = x.shape[0], x.shape[1]
    c_act = jax.nn.silu(c)
    for l in range(DEPTH):
        mod = (c_act @ w_ada[l] + b_ada[l])[:, None, :]
        sh1, sc1, g1, sh2, sc2, g2 = jnp.split(mod, 6, axis=-1)

        h = rmsnorm(x, norm_mix[l]) * (1.0 + sc1) + sh1
        proj = h @ w_in[l] + b_in[l]
        q, k, v, u = jnp.split(proj, [D_ATTN, D_ATTN + D_KV, D_ATTN + 2 * D_KV], axis=-1)
        q = rmsnorm(q.reshape(B, S, N_KV_HEADS, Q_PER_KV, HEAD_DIM), q_norm[l])
        k = rmsnorm(k.reshape(B, S, N_KV_HEADS, HEAD_DIM), k_norm[l])
        v = v.reshape(B, S, N_KV_HEADS, HEAD_DIM)
        attn = sliding_window_attention(q, k, v, sinks[l])

        ssm = s5_ssm(u, lam_re[l], lam_im[l], log_dt[l], b_re[l], b_im[l],
                     c_re[l], c_im[l], d_skip[l])
        ssm = jax.nn.gelu(ssm)
        ssm = ssm * jax.nn.sigmoid(ssm @ w_glu[l] + b_glu[l])

        mixed = jnp.concatenate([rmsnorm(attn, attn_out_norm[l]),
                                 rmsnorm(ssm, ssm_out_norm[l])], axis=-1)
        x = x + g1 * (mixed @ w_out[l])

        h2 = rmsnorm(x, norm_ffn[l]) * (1.0 + sc2) + sh2
        x = x + g2 * moe_ffn(h2, w_router[l], b_router[l], w_gate_up[l],
                             b_gate_up[l], w_down[l], b_down[l])
    return x
```

```python
import os
import math
import numpy as np
from contextlib import ExitStack
import concourse.bass as bass
import concourse.mybir as mybir
from concourse.bass_utils import run_bass_kernel_spmd

F32 = mybir.dt.float32
BF16 = mybir.dt.bfloat16
AF = mybir.ActivationFunctionType
ALU = mybir.AluOpType
AX = mybir.AxisListType

S_ = 2048
D = 1024
NT = 16
NE = 32
CAP = 768
HOT_T = [4, 4]
HOTN = len(HOT_T)
HOT0 = NE * CAP
NROWS = HOT0 + 128 * sum(HOT_T)

EPS = 1e-6
KB = 1024
MAGIC = 12582912.0
TWO_PI = 2.0 * math.pi


class _Op:
    __slots__ = ("idx", "eng", "fn", "semkey", "deps", "sig", "group")


class Sched:
    ENGS = ("pe", "act", "dve", "pool", "sp")

    def __init__(self):
        self.ops = []
        self.lastw = {}
        self.readers = {}
        self.barrier_deps = set()
        self.last_on_eng = {}
        self.last_dma = {}

    def add(self, eng, fn, reads=(), writes=(), semkey=None, group=False):
        if getattr(self, 'disabled', False):
            return None
        op = _Op()
        op.idx = len(self.ops)
        op.eng = eng
        op.fn = fn
        op.semkey = semkey
        op.group = group
        op.sig = None
        deps = set(self.barrier_deps)
        for k in reads:
            w = self.lastw.get(k)
            if w is not None:
                deps.add(w)
        for k in writes:
            w = self.lastw.get(k)
            if w is not None:
                deps.add(w)
            deps.update(self.readers.get(k, {}).values())
        if eng == "pe" and semkey is None:
            deps = {d for d in deps if not (self.ops[d].eng == "pe" and self.ops[d].semkey is None)}
        op.deps = deps
        for k in reads:
            r = self.readers.setdefault(k, {})
            rk = eng if semkey is None else ("dma", op.idx)
            r[rk] = op.idx
        for k in writes:
            self.lastw[k] = op.idx
            self.readers[k] = {}
        self.ops.append(op)
        if semkey is None:
            self.last_on_eng[eng] = op.idx
        else:
            self.last_dma[semkey] = op.idx
        return op

    def mark(self, name):
        if os.environ.get('MK_STOP', '') == name:
            self.disabled = True

    def barrier(self):
        self.barrier_deps = set(self.last_on_eng.values()) | set(self.last_dma.values())

    def emit(self, nc, stack):
        needed = set()
        for op in self.ops:
            needed |= op.deps
        sems = {}

        def getsem(name):
            if name not in sems:
                sems[name] = stack.enter_context(nc.semaphore("s_%d" % len(sems)))
            return sems[name]

        cnt = {}
        for op in self.ops:
            if op.semkey is not None:
                cnt[op.semkey] = cnt.get(op.semkey, 0) + 16
                op.sig = (op.semkey, cnt[op.semkey])
            elif op.idx in needed:
                cnt[op.eng] = cnt.get(op.eng, 0) + 1
                op.sig = (op.eng, cnt[op.eng])
        for op in self.ops:
            if op.semkey is not None and op.group:
                op.sig = (op.semkey, cnt[op.semkey])
        per_eng = {e: [] for e in self.ENGS}
        seen = {e: {} for e in self.ENGS}
        for op in self.ops:
            w = {}
            for d in op.deps:
                name, val = self.ops[d].sig
                if val > w.get(name, 0):
                    w[name] = val
            waits = []
            for name, val in w.items():
                if seen[op.eng].get(name, 0) < val:
                    seen[op.eng][name] = val
                    waits.append((name, val))
            per_eng[op.eng].append((op, waits))
        handles = {"pe": "tensor", "act": "scalar", "dve": "vector", "pool": "gpsimd", "sp": "sync"}
        for op in self.ops:
            if op.sig is not None:
                getsem(op.sig[0])
        block = stack.enter_context(nc.Block())

        def body(engname):
            def _f(eng):
                for op, waits in per_eng[engname]:
                    for name, val in waits:
                        eng.wait_ge(sems[name], val)
                    inst = op.fn(eng) if op.fn is not None else None
                    if op.sig is not None:
                        assert inst is not None
                        if op.semkey is not None:
                            inst.then_inc(sems[op.semkey], 16)
                        else:
                            inst.then_inc(sems[op.eng], 1)
            return _f

        for engname in self.ENGS:
            if per_eng[engname]:
                getattr(block, handles[engname])(body(engname))
        self.counts = cnt


C_IDENT = 0
C_SHIFT = 128
C_BONES = 256
C_NMCUR = 384
C_NMPRV = 512
C_ROWM = 640
C_SGN = 642
C_EPS = 643
C_ONE = 644
C_NHALFPI = 645
C_ROWM4 = 648
C_BD16 = 652
C_HB = 780
C_HC = 788
C_PIDX = 796
NCST = 800


def _consts():
    c = np.zeros((128, NCST), np.float32)
    c[:, C_IDENT:C_IDENT + 128] = np.eye(128)
    sh = np.zeros((128, 128), np.float32)
    for k in range(128):
        sh[k, (k + 64) % 128] = 1.0
    c[:, C_SHIFT:C_SHIFT + 128] = sh
    bo = np.zeros((128, 128), np.float32)
    bo[:64, :64] = 1.0
    bo[64:, 64:] = 1.0
    c[:, C_BONES:C_BONES + 128] = bo
    s = np.arange(128)[:, None]
    t = np.arange(128)[None, :]
    c[:, C_NMCUR:C_NMCUR + 128] = np.where(s <= t, 0.0, -30000.0)
    c[:, C_NMPRV:C_NMPRV + 128] = np.where(s > t, 0.0, -30000.0)
    p = np.arange(128)
    c[:, C_ROWM] = ((p // 16) % 2 == 0)
    c[:, C_ROWM + 1] = ((p // 16) % 2 == 1)
    c[:64, C_SGN] = 1.0
    c[64:, C_SGN] = -1.0
    c[:, C_EPS] = EPS
    c[:, C_ONE] = 1.0
    c[:, C_NHALFPI] = 0.5 * math.pi
    for a4 in range(4):
        c[:, C_ROWM4 + a4] = ((p // 16) % 4 == a4)
    c[:, C_BD16:C_BD16 + 128] = (p[:, None] // 16 == p[None, :] // 16)
    c[:, C_HB:C_HB + HOTN] = (128 * (np.cumsum([0] + HOT_T)[:HOTN]))[None, :]
    c[:, C_HC:C_HC + HOTN] = (128 * np.array(HOT_T))[None, :]
    c[:, C_PIDX] = p
    return c


P_CCOL = 0
P_BADA = 8
P_NMIX = 56
P_NFFN = 64
P_BIN = 72
P_QG = 81
P_LAMR = 83
P_LAMI = 115
P_LDT = 147
P_DSK = 179
P_BGLU = 183
P_GCAT = 187
P_SINK = 195
P_BROUT = 203
P_BVROW = 235
P_BG = 363
P_BU = 619
NPRM = 875


def build(debug=False, nexp=NE):
    nc = bass.Bass("TRN2", target_bir_lowering=False)
    dr = lambda n, s: nc.dram_tensor(n, s, F32, kind="ExternalInput").ap()
    x_d = dr("x", [S_, D])
    cst_d = dr("cst", [128, NCST])
    prm_d = dr("prm", [128, NPRM])
    wada_d = dr("w_ada", [D, 6 * D])
    badag_d = dr("bada_g", [128, 2 * D])
    win_d = dr("w_in", [D, 1280])
    sb_d = dr("ssm_sb", [128, 2, 512])
    sc_d = dr("ssm_sc", [128, 2, 512])
    wglu_d = dr("w_glu", [512, 512])
    wout_d = dr("w_out", [D, D])
    wr_d = dr("w_router", [D, NE])
    WOFF_D = 8 * D * 256
    WOFF_B = WOFF_D + D * D
    WALL = WOFF_B + 128 * 16
    wall_d = dr("wall", [NE, WALL])
    bd_d = dr("b_down", [NE, D])
    out_d = nc.dram_tensor("out", [S_, D], F32, kind="ExternalOutput").ap()
    Hs = nc.dram_tensor("hs_scr", [NROWS, D], BF16, kind="Internal").ap()
    Ys = nc.dram_tensor("ys_scr", [NROWS, D], F32, kind="Internal").ap()
    dbg = {}
    if debug:
        for n, shp in [("d_hT", [128, 8 * 2048]), ("d_qT", [128, 4 * 2048]), ("d_kT", [128, 2048]), ("d_uT", [128, 4 * 2048]),
                       ("d_v1", [128, 16 * 132]), ("d_mix", [128, 8 * 2048]), ("d_x32", [128, 32 * 257]), ("d_gT", [128, 4 * 2048]),
                       ("d_x1", [128, 16 * 1024]), ("d_G", [128, 16 * 32]), ("d_gk", [128, 64]), ("d_idx", [128, 64]), ("d_mod", [128, 64]),
                       ("d_g1", [128, 2048]), ("d_sp", [128, 9 * 32])]:
            dbg[n] = nc.dram_tensor(n, shp, F32, kind="ExternalOutput").ap()

    S = Sched()
    st = ExitStack()
    with st:
        ARW = 190 * KB // 4
        arena = st.enter_context(nc.sbuf_tensor("arena", [128, ARW], F32))
        cst = st.enter_context(nc.sbuf_tensor("cstt", [128, NCST], F32))
        prm = st.enter_context(nc.sbuf_tensor("prmt", [128, NPRM], F32))
        cbf = st.enter_context(nc.sbuf_tensor("cbf", [128, 5 * 128], BF16))
        smalls = st.enter_context(nc.sbuf_tensor("smalls", [128, 512], F32))
        Gt = st.enter_context(nc.sbuf_tensor("Gt", [128, NT, NE], F32))
        IDX = st.enter_context(nc.sbuf_tensor("idxt", [128, NT * 4], mybir.dt.int32))
        Gk = st.enter_context(nc.sbuf_tensor("gkt", [128, NT, 4], F32))
        idx8 = st.enter_context(nc.sbuf_tensor("idx8", [128, 8], mybir.dt.uint32))
        pi_i = st.enter_context(nc.sbuf_tensor("pi_i", [1, 8], mybir.dt.int32))
        hbias = st.enter_context(nc.sbuf_tensor("hbias", [128, 2, 16], F32))
        psum = st.enter_context(nc.psum_tensor("psum", [128, 4096], F32))

        def af(off, n):
            assert off % 4 == 0
            return arena[:, off // 4: off // 4 + n]

        def ab(off, n):
            assert off % 4 == 0 and n % 2 == 0
            return arena[:, off // 4: off // 4 + n // 2].bitcast(BF16)

        def bank(i):
            return psum[:, i * 512:(i + 1) * 512]

        def bankb(i):
            return bank(i).bitcast(BF16)

        PSK = lambda i: ("ps", i)
        ident_b = cbf[:, 0:128]
        shift_b = cbf[:, 128:256]
        bones_b = cbf[:, 256:384]
        ones_b = cbf[:, 512:640]
        identf = cst[:, C_IDENT:C_IDENT + 128]
        shiftf = cst[:, C_SHIFT:C_SHIFT + 128]
        epsc = cst[:, C_EPS:C_EPS + 1]
        sgn = cst[:, C_SGN:C_SGN + 1]

        def act(out, in_, func, r, w, scale=None, bias=None, accum=None):
            kw = {}
            if scale is not None:
                kw["scale"] = scale
            if bias is not None:
                kw["bias"] = bias
            if accum is not None:
                kw["accum_out"] = accum
            S.add("act", lambda e: e.activation(out=out, in_=in_, func=func, **kw), reads=r, writes=w)

        def tt(eng, out, in0, in1, op, r, w):
            S.add(eng, lambda e: e.tensor_tensor(out=out, in0=in0, in1=in1, op=op), reads=r, writes=w)

        def ts(eng, out, in0, s1, op0, r, w, s2=None, op1=None):
            if op1 is None:
                S.add(eng, lambda e: e.tensor_scalar(out=out, in0=in0, scalar1=s1, scalar2=None, op0=op0), reads=r, writes=w)
            else:
                S.add(eng, lambda e: e.tensor_scalar(out=out, in0=in0, scalar1=s1, scalar2=s2, op0=op0, op1=op1), reads=r, writes=w)

        def stt(out, in0, scalar, in1, op0, op1, r, w):
            S.add("dve", lambda e: e.scalar_tensor_tensor(out=out, in0=in0, scalar=scalar, in1=in1, op0=op0, op1=op1), reads=r, writes=w)

        def cp(eng, out, in_, r, w):
            if eng == "act":
                S.add(eng, lambda e: e.activation(out=out, in_=in_, func=AF.Identity), reads=r, writes=w)
            else:
                S.add(eng, lambda e: e.tensor_copy(out=out, in_=in_), reads=r, writes=w)

        def mm(out, lhsT, rhs, start, stop, r, w):
            S.add("pe", lambda e: e.matmul(out, lhsT=lhsT, rhs=rhs, start=start, stop=stop), reads=r, writes=w)

        def tr(out, in_, idn, r, w):
            S.add("pe", lambda e: e.transpose(out, in_, idn), reads=r, writes=w)

        def dma(q, out, in_, r, w, semkey, group=False):
            S.add(q, lambda e: e.dma_start(out=out, in_=in_), reads=r, writes=w, semkey=semkey, group=group)

        def memset(eng, ap, val, w):
            S.add(eng, lambda e: e.memset(ap, val), writes=w)

        regcache = {}

        def bnd(e):
            if "r" not in regcache:
                regcache["r"] = e.to_reg(NROWS - 1)
            return regcache["r"]

        def dump(name, ap, r):
            if debug:
                dma("pool", dbg[name], ap, r, ["dbg_" + name], "dbg_" + name)

        R1, R2, R3, R4 = 0, 64 * KB, 96 * KB, 140 * KB
        SB = af(R1 + 0, 512); SBp = af(R1 + 2 * KB, 512); SC = af(R1 + 4 * KB, 512); SCp = af(R1 + 6 * KB, 512)
        S0 = af(R1 + 8 * KB, 512); S0p = af(R1 + 10 * KB, 512); T1 = af(R1 + 12 * KB, 512); T2 = af(R1 + 14 * KB, 512)
        PBk = ab(R1 + 16 * KB, 8 * 512).rearrange("p (k c) -> p k c", c=512)
        qT = ab(R1 + 0, 4 * 2048).rearrange("p (g t) -> p g t", t=2048)
        kT = ab(R1 + 16 * KB, 2048)
        v1 = ab(R1 + 20 * KB, 16 * 2 * 66).rearrange("p (t k c) -> p t k c", k=2, c=66)
        uT = ab(R1 + 25 * KB, 4 * 2048).rearrange("p (j t) -> p j t", t=2048)
        gT = qT
        TB = R1 + 41 * KB
        xres = af(R1, NT * D).rearrange("p (t d) -> p t d", d=D)
        hT = ab(R2, 8 * 2048).rearrange("p (k t) -> p k t", t=2048)
        mixT = hT
        actT = hT
        Qd = ab(R3, 9 * 512).rearrange("p (k c) -> p k c", c=512)
        PTp = ab(R3 + 9 * KB, 4 * 4 * 8 * 128).rearrange("p (j a k c) -> p j a k c", a=4, k=8, c=128)
        MT = ab(R1 + 56 * KB, 4 * 8 * 128).rearrange("p (j k c) -> p j k c", k=8, c=128)
        s1L = af(R3 + 41 * KB, 256).rearrange("p (l g) -> p l g", g=32)
        s2L = af(R3 + 42 * KB, 256).rearrange("p (l g) -> p l g", g=32)
        slab = [ab(R3 + i * 4 * KB, 8 * 256).rearrange("p (k c) -> p k c", c=256) for i in range(3)]
        wdb = [ab(R3 + 12 * KB + i * 16 * KB, 8 * 1024).rearrange("p (k c) -> p k c", c=1024) for i in range(2)]

        dma("sp", cst[:], cst_d, [], ["cst"], "cstld", group=True)
        dma("sp", prm[:], prm_d, [], ["prm"], "cstld", group=True)
        dma("sp", SB, sb_d[:, 0, :], [], ["SB"], "cstld", group=True)
        dma("sp", SBp, sb_d[:, 1, :], [], ["SBp"], "cstld", group=True)
        dma("sp", SC, sc_d[:, 0, :], [], ["SC"], "cstld", group=True)
        dma("sp", SCp, sc_d[:, 1, :], [], ["SCp"], "cstld", group=True)
        win_b = ab(R4, 8 * 1280).rearrange("p (k c) -> p k c", c=1280)
        wada = af(TB, 8 * 256).rearrange("p (k c) -> p k c", c=256)
        xt = [af(R4 + 20 * KB + i * 4 * KB, 1024) for i in range(2)]
        xn = [af(R4 + 28 * KB + i * 4 * KB, 1024) for i in range(2)]
        qraw = [af(R4 + 36 * KB + i * 2 * KB, 512) for i in range(2)]
        sqb = [ab(R4 + 40 * KB + i * KB, 512) for i in range(2)]
        lnv = af(R4 + 42 * KB, 512)
        T_junk = ab(R4 + 44 * KB, 1024)
        lnv2 = [lnv, af(R4 + 46 * KB, 512)]
        nmask = ab(TB + 12 * KB, 2 * 512).rearrange("p (a c) -> p a c", c=512)
        dma("pool", win_b, win_d.rearrange("(k p) c -> p k c", p=128), [], ["win"], "winld")

        cp("dve", cbf[:, 0:384], cst[:, 0:384], ["cst"], ["cbf"])
        memset("dve", cbf[:, 512:640], 1.0, ["cbf1"])
        SM = lambda a, n: smalls[:, a:a + n]
        cact = SM(0, 8)
        modc = SM(8, 32).rearrange("p (s k) -> p s k", k=8)
        scale1 = SM(40, 8); scale2 = SM(48, 8)
        esink = SM(56, 8)
        bu1 = None
        ssq_a = SM(64, 16); std_a = SM(80, 16); rstd_a = SM(96, 16)
        ssq_b = SM(112, 16); std_b = SM(128, 16); rstd_b = SM(144, 16)
        ssq_d = SM(160, 16); std_d = SM(176, 16); rstd_d = SM(192, 16)
        den4 = SM(208, 8); rden4 = SM(216, 8)
        rt = SM(224, 64)
        cb2 = SM(288, 16)

        act(cact, prm[:, P_CCOL:P_CCOL + 8], AF.Silu, ["prm"], ["cact"])
        cact2 = SM(400, 16).rearrange("p (k c) -> p k c", c=2)
        cp("dve", cact2, cact.unsqueeze(2).broadcast_to([128, 8, 2]), ["cact"], ["cact"])
        act(esink, prm[:, P_SINK:P_SINK + 8], AF.Exp, ["prm"], ["esink"])

        def wada_load(sec, q):
            c0 = sec * 1024 + q * 256
            dma("sp", wada, wada_d[:, c0:c0 + 256].rearrange("(k p) c -> p k c", p=128), [], ["wada"], "wadald")

        cbT = af(TB + 8 * KB, 8 * 128).rearrange("p (k m) -> p k m", m=128)
        cp("dve", cbT, cact.unsqueeze(2).broadcast_to([128, 8, 128]), ["cact"], ["cbT"])

        def mod_cols(sec, slot, fin=True):
            for q in range(4):
                wada_load(sec, q)
                for fc in range(2):
                    col = slot * 8 + q * 2 + fc
                    for kc in range(8):
                        mm(bank(6)[:, 2 * col:2 * col + 2], wada[:, kc, fc * 128:(fc + 1) * 128], cact2[:, kc, :], kc == 0, kc == 7,
                           ["wada", "cact"], [PSK(6)])
            def _fin():
                tt("dve", modc[:, slot, :], bank(6)[:, slot * 16:slot * 16 + 16:2], prm[:, P_BADA + sec * 8:P_BADA + sec * 8 + 8], ALU.add,
                   [PSK(6), "prm"], ["modc%d" % slot])
            if fin:
                _fin()
            return _fin

        fin0 = mod_cols(0, 0, fin=False)
        fin1 = mod_cols(1, 1, fin=False)

        sw = af(R2, 32 * 40).rearrange("p (a g) -> p a g", g=32)
        SP1 = af(R2 + 5 * KB, 9 * 32).rearrange("p (k g) -> p k g", g=32)
        SP2 = af(R2 + 7 * KB, 9 * 32).rearrange("p (k g) -> p k g", g=32)
        SP1q = af(R2 + 9 * KB, 9 * 32).rearrange("p (k g) -> p k g", g=32)
        SP2q = af(R2 + 11 * KB, 9 * 32).rearrange("p (k g) -> p k g", g=32)
        lamr = prm[:, P_LAMR:P_LAMR + 32]
        lami = prm[:, P_LAMI:P_LAMI + 32]
        K_ = "ssmsetup"
        dt_ = sw[:, 0, :]; aa = sw[:, 1, :]; mag = sw[:, 2, :]; ang = sw[:, 3, :]
        act(dt_, prm[:, P_LDT:P_LDT + 32], AF.Exp, ["prm"], [K_])
        tt("dve", aa, lamr, dt_, ALU.mult, ["prm", K_], [K_])
        act(mag, aa, AF.Exp, [K_], [K_])
        tt("dve", ang, lami, dt_, ALU.mult, ["prm", K_], [K_])

        def sincos(dst, shift, base):
            t0 = sw[:, base, :]; t1 = sw[:, base + 1, :]; t2 = sw[:, base + 2, :]; t3 = sw[:, base + 3, :]
            ts("dve", t0, ang, shift, ALU.add, [K_], [K_])
            ts("dve", t1, t0, 1.0 / TWO_PI, ALU.mult, [K_], [K_])
            ts("dve", t2, t1, MAGIC, ALU.add, [K_], [K_])
            ts("dve", t2, t2, -MAGIC, ALU.add, [K_], [K_])
            stt(t3, t2, -TWO_PI, t0, ALU.mult, ALU.add, [K_], [K_])
            ts("dve", t3, t3, 3.14159, ALU.min, [K_], [K_], s2=-3.14159, op1=ALU.max)
            act(dst, t3, AF.Sin, [K_], [K_])

        sn = sw[:, 4, :]; cs = sw[:, 5, :]
        sincos(sn, 0.0, 8)
        sincos(cs, 0.5 * math.pi, 12)
        lbr = SP1[:, 1, :]; lbi_raw = sw[:, 6, :]
        tt("dve", lbr, mag, cs, ALU.mult, [K_], [K_])
        tt("dve", lbi_raw, mag, sn, ALU.mult, [K_], [K_])
        ts("dve", SP2[:, 1, :], lbi_raw, sgn, ALU.mult, [K_, "cst"], [K_])
        den = sw[:, 16, :]; t_a = sw[:, 17, :]; t_b = sw[:, 18, :]; rden = sw[:, 19, :]; lbm1 = sw[:, 20, :]
        cr = sw[:, 21, :]; ci = sw[:, 22, :]; c2 = sw[:, 23, :]
        tt("dve", t_a, lamr, lamr, ALU.mult, ["prm"], [K_])
        tt("dve", t_b, lami, lami, ALU.mult, ["prm", K_], [K_])
        tt("dve", den, t_a, t_b, ALU.add, [K_], [K_])
        S.add("dve", lambda e: e.reciprocal(out=rden, in_=den), reads=[K_], writes=[K_])
        ts("dve", lbm1, lbr, -1.0, ALU.add, [K_], [K_])
        tt("dve", t_a, lbm1, lamr, ALU.mult, [K_, "prm"], [K_])
        tt("dve", t_b, lbi_raw, lami, ALU.mult, [K_, "prm"], [K_])
        tt("dve", t_a, t_a, t_b, ALU.add, [K_], [K_])
        tt("dve", cr, t_a, rden, ALU.mult, [K_], [K_])
        tt("dve", t_a, lbi_raw, lamr, ALU.mult, [K_, "prm"], [K_])
        tt("dve", t_b, lbm1, lami, ALU.mult, [K_, "prm"], [K_])
        tt("dve", t_a, t_a, t_b, ALU.subtract, [K_], [K_])
        tt("dve", ci, t_a, rden, ALU.mult, [K_], [K_])
        ts("dve", c2, ci, sgn, ALU.mult, [K_, "cst"], [K_])

        def bc(v):
            return v.unsqueeze(2).broadcast_to([128, 32, 16])

        v3 = lambda a: a.rearrange("p (g h) -> p g h", h=16)
        tt("dve", v3(T1), v3(SB), bc(cr), ALU.mult, ["SB", K_], [K_])
        tt("dve", v3(T2), v3(SBp), bc(c2), ALU.mult, ["SBp", K_], [K_])
        tt("dve", S0, T1, T2, ALU.subtract, [K_], [K_])
        tt("dve", v3(T1), v3(SBp), bc(cr), ALU.mult, ["SBp", K_], [K_])
        tt("dve", v3(T2), v3(SB), bc(c2), ALU.mult, ["SB", K_], [K_])
        tt("dve", S0p, T1, T2, ALU.add, [K_], [K_])
        memset("dve", SP1[:, 0, :], 1.0, [K_])
        memset("dve", SP2[:, 0, :], 0.0, [K_])
        for k in range(1, 8):
            tt("dve", t_a, SP1[:, k, :], SP1[:, 1, :], ALU.mult, [K_], [K_])
            tt("dve", t_b, SP2[:, k, :], SP2[:, 1, :], ALU.mult, [K_], [K_])
            tt("dve", SP1[:, k + 1, :], t_a, t_b, ALU.subtract, [K_], [K_])
            tt("dve", t_a, SP1[:, k, :], SP2[:, 1, :], ALU.mult, [K_], [K_])
            tt("dve", t_b, SP2[:, k, :], SP1[:, 1, :], ALU.mult, [K_], [K_])
            tt("dve", SP2[:, k + 1, :], t_a, t_b, ALU.add, [K_], [K_])
        ts("dve", SP1q.rearrange("p k g -> p (k g)"), SP1.rearrange("p k g -> p (k g)"), sgn, ALU.mult, [K_, "cst"], [K_])
        ts("dve", SP2q.rearrange("p k g -> p (k g)"), SP2.rearrange("p k g -> p (k g)"), sgn, ALU.mult, [K_, "cst"], [K_])
        cp("dve", s1L[:, 0, :], SP1[:, 8, :], [K_], ["sL"])
        cp("dve", s2L[:, 0, :], SP2[:, 8, :], [K_], ["sL"])
        for l in range(7):
            tt("dve", t_a, s1L[:, l, :], s1L[:, l, :], ALU.mult, ["sL", K_], [K_])
            tt("dve", t_b, s2L[:, l, :], s2L[:, l, :], ALU.mult, ["sL", K_], [K_])
            tt("dve", s1L[:, l + 1, :], t_a, t_b, ALU.subtract, [K_], ["sL"])
            tt("dve", t_a, s1L[:, l, :], s2L[:, l, :], ALU.mult, ["sL", K_], [K_])
            ts("dve", s2L[:, l + 1, :], t_a, 2.0, ALU.mult, [K_], ["sL"])
        dump("d_sp", SP1.rearrange("p k g -> p (k g)"), [K_])
        for k in range(9):
            if k < 8:
                tt("dve", v3(T1), v3(S0), bc(SP1[:, k, :]), ALU.mult, [K_], [K_])
                tt("dve", v3(T2), v3(S0p), bc(SP2[:, k, :]), ALU.mult, [K_], [K_])
                tt("dve", T1, T1, T2, ALU.subtract, [K_], [K_])
                cp("act", PBk[:, k, :], T1, [K_], ["PBk"])
            tt("dve", v3(T1), v3(SC), bc(SP1q[:, k, :]), ALU.mult, ["SC", K_], [K_])
            tt("dve", v3(T2), v3(SCp), bc(SP2q[:, k, :]), ALU.mult, ["SCp", K_], [K_])
            tt("dve", T1, T1, T2, ALU.subtract, [K_], [K_])
            cp("act", Qd[:, k, :], T1, [K_], ["Qd"])
        for j in range(4):
            for k in range(8):
                bi = (j * 8 + k) % 2
                tr(bankb(bi)[:, 0:128], PBk[:, k, j * 128:(j + 1) * 128], ident_b, ["PBk", "cbf"], [PSK(bi)])
                for a_ in range(4):
                    ts("dve", PTp[:, j, a_, k, :], bankb(bi)[:, 0:128], cst[:, C_ROWM4 + a_:C_ROWM4 + a_ + 1], ALU.mult,
                       [PSK(bi), "cst"], ["PTp"])
        for j in range(4):
            jsl = slice(j * 128, (j + 1) * 128)
            for dl in range(8):
                bi = 2 + (j * 8 + dl) % 2
                mm(bank(bi)[:, 0:128], PBk[:, dl, jsl], Qd[:, 0, jsl], True, True, ["PBk", "Qd"], [PSK(bi)])
                tt("dve", MT[:, j, dl, :], bank(bi)[:, 0:128], cst[:, C_BD16:C_BD16 + 128], ALU.mult, [PSK(bi), "cst"], ["MT"])
                if dl == 0:
                    stt(MT[:, j, 0, :], identf, prm[:, P_DSK + j:P_DSK + j + 1], MT[:, j, 0, :], ALU.mult, ALU.add, ["MT", "cst", "prm"], ["MT"])

        fin0()
        fin1()
        stt(scale1, modc[:, 1, :], 1.0, prm[:, P_NMIX:P_NMIX + 8], ALU.add, ALU.mult, ["modc1", "prm"], ["scale1"])
        S.mark('setup')
        S.barrier()
        S.mark('ada')
        memset("pool", v1[:, :, :, 64:65], 1.0, ["v1ones"])

        def norm_tile(t, xsrc, rk, ssq, std, rstd, scale, bias, dstT, dst_dt_key, xnbuf, pbanks, extra=None):
            act(T_junk, xsrc, AF.Square, rk, ["junk"], accum=ssq[:, t:t + 1])
            act(std[:, t:t + 1], ssq[:, t:t + 1], AF.Sqrt, ["junk"], ["std"], scale=1.0 / D, bias=epsc)
            S.add("dve", lambda e: e.reciprocal(out=rstd[:, t:t + 1], in_=std[:, t:t + 1]), reads=["std"], writes=["rstd"])
            ts("dve", xnbuf, xsrc, rstd[:, t:t + 1], ALU.mult, rk + ["rstd"], [("xn", id(xnbuf))])
            for kc in range(8):
                b_ = pbanks[kc // 4]
                tr(bank(b_)[:, (kc % 4) * 128:(kc % 4) * 128 + 128], xnbuf[:, kc * 128:(kc + 1) * 128], identf,
                   [("xn", id(xnbuf)), "cst"], [PSK(b_)])

        bias1 = modc[:, 0, :]
        NCH = 9
        for t in range(NT):
            b = t % 2
            dma("sp", xt[b], x_d[t * 128:(t + 1) * 128, :], [], [("xt", b)], "xtld%d" % b)
            pb = (0, 1) if b == 0 else (2, 3)
            norm_tile(t, xt[b], [("xt", b)], ssq_a, std_a, rstd_a, scale1, bias1, hT, None, xn[b], pb)
            tb = t // 4
            for kc in range(8):
                b_ = pb[kc // 4]
                act(hT[:, kc, t * 128:(t + 1) * 128], bank(b_)[:, (kc % 4) * 128:(kc % 4) * 128 + 128], AF.Identity,
                    [PSK(b_), "scale1", "modc0"], [("hT", tb)], scale=scale1[:, kc:kc + 1], bias=bias1[:, kc:kc + 1])
            for kc in range(8):
                mm(bank(7)[:, 0:128], hT[:, kc, t * 128:(t + 1) * 128], win_b[:, kc, 1152:1280], kc == 0, kc == 7,
                   [("hT", tb), "win"], [PSK(7)])
            tt("dve", v1[:, t, :, 0:64], bank(7)[:, 0:128].rearrange("p (k c) -> p k c", c=64),
               prm[:, P_BVROW:P_BVROW + 128].rearrange("p (k c) -> p k c", c=64), ALU.add, [PSK(7), "prm"], [("v1", t)])
            if t % 4 == 3:
                tsl = slice(tb * 512, tb * 512 + 512)
                def s0(oc):
                    pbk = 4 + oc % 2
                    for kc in range(8):
                        mm(bank(pbk), win_b[:, kc, oc * 128:(oc + 1) * 128], hT[:, kc, tsl], kc == 0, kc == 7, [("hT", tb), "win"], [PSK(pbk)])

                def s1(oc):
                    pbk = 4 + oc % 2
                    bcol = prm[:, P_BIN + oc:P_BIN + oc + 1]
                    if oc < 5:
                        qi = oc % 2
                        act(qraw[qi], bank(pbk), AF.Identity, [PSK(pbk), "prm"], [("qraw", qi)], bias=bcol)
                        act(sqb[qi], bank(pbk), AF.Square, [PSK(pbk), "prm"], [("sqb", qi)], bias=bcol)
                        mm(bank(6 + qi), bones_b, sqb[qi], True, True, [("sqb", qi), "cbf"], [PSK(6 + qi)])
                    else:
                        act(uT[:, oc - 5, tsl], bank(pbk), AF.Identity, [PSK(pbk), "prm"], [("uT", tb)], bias=bcol)

                def s2(oc):
                    if oc >= 5:
                        return
                    qi = oc % 2
                    lv = lnv2[qi]
                    act(lv, bank(6 + qi), AF.Ln, [PSK(6 + qi)], [("lnv", qi)], scale=1.0 / 64, bias=epsc)
                    act(lv, lv, AF.Exp, [("lnv", qi)], [("lnv", qi)], scale=-0.5)
                    dst = qT[:, oc, tsl] if oc < 4 else kT[:, tsl]
                    gcol = prm[:, P_QG:P_QG + 1] if oc < 4 else prm[:, P_QG + 1:P_QG + 2]
                    stt(dst, qraw[qi], gcol, lv, ALU.mult, ALU.mult, [("qraw", qi), ("lnv", qi), "prm"], [("qk", oc, tb)])

                for step in range(NCH + 2):
                    if step < NCH:
                        s0(step)
                    if 0 <= step - 1 < NCH:
                        s1(step - 1)
                    if 0 <= step - 2 < NCH:
                        s2(step - 2)
        dump("d_hT", hT.rearrange("p k t -> p (k t)"), [("hT", i) for i in range(4)])
        dump("d_qT", qT.rearrange("p g t -> p (g t)"), [("qk", oc, tb) for oc in range(4) for tb in range(4)])
        dump("d_kT", kT, [("qk", 4, tb) for tb in range(4)])
        dump("d_uT", uT.rearrange("p j t -> p (j t)"), [("uT", i) for i in range(4)])
        dump("d_v1", v1.rearrange("p t k c -> p (t k c)"), [("v1", t) for t in range(NT)] + ["v1ones"])

        grow = st.enter_context(nc.sbuf_tensor("grow", [128, 2, 1024], BF16))
        wadaL = af(R4, 8 * 256).rearrange("p (k c) -> p k c", c=256)
        cbTL = af(R4 + 8 * KB, 8 * 128).rearrange("p (k m) -> p k m", m=128)
        bgrL = af(R4 + 12 * KB, 256)
        late_units = []

        def _wl(sec, q):
            c0 = sec * 1024 + q * 256
            dma("sp", wadaL, wada_d[:, c0:c0 + 256].rearrange("(k p) c -> p k c", p=128), [], ["wadaL"], "wadaLld")

        def _mk_col_unit(sec, slot, q):
            def u():
                if sec == 3 and q == 0:
                    cp("dve", cbTL, cact.unsqueeze(2).broadcast_to([128, 8, 128]), ["cact"], ["cbTL"])
                _wl(sec, q)
                for fc in range(2):
                    col = slot * 8 + q * 2 + fc
                    for kc in range(8):
                        mm(bank(7)[:, 256 + 2 * col:256 + 2 * col + 2], wadaL[:, kc, fc * 128:(fc + 1) * 128], cact2[:, kc, :], kc == 0, kc == 7,
                           ["wadaL", "cact"], [PSK(7)])
                if q == 3:
                    tt("dve", modc[:, slot, :], bank(7)[:, 256 + slot * 16:256 + slot * 16 + 16:2], prm[:, P_BADA + sec * 8:P_BADA + sec * 8 + 8],
                       ALU.add, [PSK(7), "prm"], ["modc%d" % slot, PSK(7)])
                    if slot == 3:
                        stt(scale2, modc[:, 3, :], 1.0, prm[:, P_NFFN:P_NFFN + 8], ALU.add, ALU.mult, ["modc3", "prm"], ["scale2"])
            return u

        def _mk_row_unit(gi_, sec, q):
            def u():
                _wl(sec, q)
                dma("sp", bgrL, badag_d[:, gi_ * 1024 + q * 256: gi_ * 1024 + q * 256 + 256], [], ["bgrL"], "bgrLld")
                for kc in range(8):
                    mm(bank(6)[:, 256:512], cbTL[:, kc, :], wadaL[:, kc, :], kc == 0, kc == 7, ["wadaL", "cbTL"], [PSK(6)])
                tt("dve", grow[:, gi_, q * 256:(q + 1) * 256], bank(6)[:, 256:512], bgrL, ALU.add, [PSK(6), "bgrL"], [("grow", gi_), PSK(6)])
            return u
        for sec, slot in ((3, 2), (4, 3)):
            for q in range(4):
                late_units.append(_mk_col_unit(sec, slot, q))
        for gi_, sec in enumerate((2, 5)):
            for q in range(4):
                late_units.append(_mk_row_unit(gi_, sec, q))

        S.mark('A')
        S.barrier()
        for a_ in range(2):
            cp("dve", nmask[:, a_, :].rearrange("p (h t) -> p h t", t=128),
               cst[:, C_NMCUR + 128 * a_: C_NMCUR + 128 * a_ + 128].unsqueeze(1).broadcast_to([128, 4, 128]), ["cst"], ["nmask"])

        Eb = [ab(TB + i * KB, 512) for i in range(4)]
        attn_f = [af(TB + 4 * KB + i * 2 * KB, 512) for i in range(2)]
        attn_b = [ab(TB + 8 * KB + i * KB, 512) for i in range(2)]
        T_junk2 = ab(TB + 10 * KB, 512)
        ecount = 0
        for n in range(NT):
            ab_ = n % 2
            for kv in range(2):
                prt = slice(kv * 64, kv * 64 + 64)
                blocks = [(n, 0)] + ([(n - 1, 1)] if n > 0 else [])
                ebufs = []
                for (kb, mi) in blocks:
                    pbk = ecount % 4
                    eb = Eb[ecount % 4]
                    ecount += 1
                    mm(bank(pbk), kT[prt, kb * 128:(kb + 1) * 128], qT[prt, :, n * 128:(n + 1) * 128], True, False,
                       [("qk", 4, kb // 4)] + [("qk", oc, n // 4) for oc in range(4)], [PSK(pbk)])
                    mm(bank(pbk), ident_b, nmask[:, mi, :], False, True, ["cbf", "nmask"], [PSK(pbk)])
                    act(eb, bank(pbk), AF.Exp, [PSK(pbk)], [("E", id(eb))], scale=0.125)
                    ebufs.append((eb, kb))
                pob = 4 + (n * 2 + kv) % 2
                po = bank(pob)[:, 0:4 * 66].rearrange("p (h c) -> p h c", c=66)
                for h in range(4):
                    for i_, (eb, kb) in enumerate(ebufs):
                        mm(po[:, h, 0:65], eb[:, h * 128:(h + 1) * 128], v1[:, kb, kv, 0:65], i_ == 0, i_ == len(ebufs) - 1,
                           [("E", id(eb)), ("v1", kb), "v1ones"], [PSK(pob)])
                dk = den4[:, kv * 4:kv * 4 + 4]
                rk_ = rden4[:, kv * 4:kv * 4 + 4]
                tt("dve", dk, po[:, :, 64], esink[:, kv * 4:kv * 4 + 4], ALU.add, [PSK(pob), "esink"], [("den", kv)])
                S.add("dve", lambda e, dk=dk, rk_=rk_: e.reciprocal(out=rk_, in_=dk), reads=[("den", kv)], writes=[("rden", kv)])
                tt("dve", attn_f[ab_][:, kv * 256:(kv + 1) * 256].rearrange("p (h c) -> p h c", c=64), po[:, :, 0:64],
                   rk_.unsqueeze(2).broadcast_to([128, 4, 64]), ALU.mult, [PSK(pob), ("rden", kv)], [("attnf", ab_)])
            act(T_junk2, attn_f[ab_], AF.Square, [("attnf", ab_)], ["junk2"], accum=ssq_b[:, n:n + 1])
            act(std_b[:, n:n + 1], ssq_b[:, n:n + 1], AF.Sqrt, ["junk2"], ["std_b"], scale=1.0 / 512, bias=epsc)
            S.add("dve", lambda e, n=n: e.reciprocal(out=rstd_b[:, n:n + 1], in_=std_b[:, n:n + 1]), reads=["std_b"], writes=["rstd_b"])
            ts("dve", attn_b[ab_], attn_f[ab_], rstd_b[:, n:n + 1], ALU.mult, [("attnf", ab_), "rstd_b"], [("attnb", ab_)])
            tbk = 6 + n % 2
            for c4 in range(4):
                tr(bankb(tbk)[:, c4 * 128:(c4 + 1) * 128], attn_b[ab_][:, c4 * 128:(c4 + 1) * 128], ident_b, [("attnb", ab_), "cbf"], [PSK(tbk)])
            cp("act", mixT[:, 0:4, n * 128:(n + 1) * 128], bankb(tbk)[:, 0:512].rearrange("p (c t) -> p c t", t=128), [PSK(tbk)], [("mixA", n)])
            late_units[n]()
        dump("d_mod", smalls[:, 0:64], ["modc0", "modc1", "modc2", "modc3", "scale1", "scale2", "cact"])
        dump("d_g1", grow[:].rearrange("p a c -> p (a c)"), [("grow", 0), ("grow", 1)])

        S.mark('B')
        S.barrier()
        X32 = af(R4, 32 * 257).rearrange("p (g c) -> p g c", c=257)
        Xbf = ab(R4 + 33 * KB, 32 * 258).rearrange("p (g c) -> p g c", c=258)
        Abuf = [ab(TB + i * 2 * KB, 8 * 128).rearrange("p (g c) -> p g c", c=128) for i in range(4)]
        At1g = [af(TB + 8 * KB + i * 512, 128) for i in range(4)]
        wglu_b = ab(TB + 10 * KB, 4 * 512).rearrange("p (k c) -> p k c", c=512)
        sigb = [ab(R1 + 19 * KB + i * KB, 512) for i in range(2)]
        sq4 = ab(R1 + 21 * KB, 4 * 512).rearrange("p (k c) -> p k c", c=512)
        rstd_c = af(R1 + 16 * KB, 512)
        Ytok = [ab(R1 + 18 * KB + i * 256, 128) for i in range(2)]
        dma("pool", wglu_b, wglu_d.rearrange("(k p) c -> p k c", p=128), [], ["wglu"], "wgluld")
        memset("dve", X32[:, :, 0:1], 0.0, ["X32z"])
        memset("dve", Xbf[:, :, 0:1], 0.0, ["Xbfz"])
        for g in range(32):
            j, gi = g // 8, g % 8
            hf, a_ = gi // 4, gi % 4
            pbk = g % 4
            prt = slice(64 * hf, 64 * hf + 64)
            for tau in range(8):
                mm(bank(pbk)[:, 0:256], PTp[prt, j, a_, 7 - tau, :], uT[prt, j, tau:2048:8], tau == 0, tau == 7,
                   ["PTp"] + [("uT", i) for i in range(4)], [PSK(pbk)])
            cp("act", X32[:, g, 1:257], bank(pbk)[:, 0:256], [PSK(pbk)], [("X32", j)])
            cp("dve", Xbf[:, g, 1:257], X32[:, g, 1:257], [("X32", j)], [("Xbf", j)])
        ps_sets = [psum[:, 0:2048].rearrange("p (g c) -> p g c", c=256), psum[:, 2048:4096].rearrange("p (g c) -> p g c", c=256)]
        for l in range(8):
            d_ = 1 << l
            for j in range(4):
                A_ = Abuf[j]
                for gi in range(8):
                    g = 8 * j + gi
                    a1 = At1g[gi % 4]
                    act(a1, identf, AF.Identity, ["cst", "sL"], [("At1g", gi % 4)], scale=s1L[:, l, g:g + 1])
                    stt(A_[:, gi, :], shiftf, s2L[:, l, g:g + 1], a1, ALU.mult, ALU.add, ["cst", "sL", ("At1g", gi % 4)], [("A", j)])
            for j in range(4):
                pset = ps_sets[j % 2]
                b0 = 4 * (j % 2)
                for gi in range(8):
                    g = 8 * j + gi
                    mm(pset[:, gi, 0:256 - d_], Abuf[j][:, gi, :], Xbf[:, g, 1:257 - d_], True, True, [("A", j), ("Xbf", j)],
                       [PSK(b0 + gi // 2)])
                tt("dve", X32[:, 8 * j:8 * j + 8, 1 + d_:257], X32[:, 8 * j:8 * j + 8, 1 + d_:257], pset[:, :, 0:256 - d_], ALU.add,
                   [PSK(b0), PSK(b0 + 1), PSK(b0 + 2), PSK(b0 + 3), ("X32", j)], [("X32", j)])
                cp("act", Xbf[:, 8 * j:8 * j + 8, 1 + d_:257], X32[:, 8 * j:8 * j + 8, 1 + d_:257], [("X32", j)], [("Xbf", j)])
        dump("d_x32", X32.rearrange("p g c -> p (g c)"), [("X32", j) for j in range(4)])
        yc = 0
        for j in range(4):
            for tau in range(8):
                pbk = yc % 4
                yc += 1
                for sg_ in range(tau + 1):
                    mm(bank(pbk)[:, 0:256], MT[:, j, tau - sg_, :], uT[:, j, sg_:2048:8], sg_ == 0, False,
                       ["MT"] + [("uT", i) for i in range(4)], [PSK(pbk)])
                for hf in range(2):
                    pt_ = 4 + (yc * 2 + hf) % 4
                    yt = Ytok[(yc * 2 + hf) % 2]
                    for gi in range(8):
                        g = 8 * j + gi
                        mm(bank(pt_)[:, gi * 16:(gi + 1) * 16], Xbf[:, g, hf * 128:(hf + 1) * 128], Qd[:, tau + 1, g * 16:(g + 1) * 16], True, True,
                           ["Qd", ("Xbf", j), "Xbfz"], [PSK(pt_)])
                    cp("dve", yt, bank(pt_)[:, 0:128], [PSK(pt_)], [("Ytok", id(yt))])
                    mm(bank(pbk)[:, hf * 128:(hf + 1) * 128], yt, ident_b, False, hf == 1, [("Ytok", id(yt)), "cbf"], [PSK(pbk)])
                act(gT[:, j, tau:2048:8], bank(pbk)[:, 0:256], AF.Gelu_apprx_tanh, [PSK(pbk)], [("gT", j)])
        dump("d_gT", gT.rearrange("p j t -> p (j t)"), [("gT", j) for j in range(4)])
        gc_ = 0
        for tb in range(4):
            tsl = slice(tb * 512, tb * 512 + 512)
            for oc in range(4):
                pbk = 4 + gc_ % 2
                sb_ = sigb[gc_ % 2]
                gc_ += 1
                for kc in range(4):
                    mm(bank(pbk), wglu_b[:, kc, oc * 128:(oc + 1) * 128], gT[:, kc, tsl], kc == 0, kc == 3,
                       ["wglu"] + [("gT", j) for j in range(4)], [PSK(pbk)])
                act(sb_, bank(pbk), AF.Sigmoid, [PSK(pbk), "prm"], [("sig", id(sb_))], bias=prm[:, P_BGLU + oc:P_BGLU + oc + 1])
                tt("dve", mixT[:, 4 + oc, tsl], gT[:, oc, tsl], sb_, ALU.mult, [("sig", id(sb_)), ("gT", oc)], [("mixS", tb)])
            act(sq4, mixT[:, 4:8, tsl], AF.Square, [("mixS", tb)], ["sq4"])
            for oc in range(4):
                mm(bank(6), ones_b, sq4[:, oc, :], oc == 0, oc == 3, ["sq4", "cbf1"], [PSK(6)])
            act(rstd_c, bank(6), AF.Ln, [PSK(6)], ["rstd_c"], scale=1.0 / 512, bias=epsc)
            act(rstd_c, rstd_c, AF.Exp, ["rstd_c"], ["rstd_c"], scale=-0.5)
            tt("dve", mixT[:, 4:8, tsl], mixT[:, 4:8, tsl], rstd_c.unsqueeze(1).broadcast_to([128, 4, 512]), ALU.mult,
               [("mixS", tb), "rstd_c"], [("mixS", tb)])
        dump("d_mix", mixT.rearrange("p k t -> p (k t)"), [("mixA", n) for n in range(NT)] + [("mixS", tb) for tb in range(4)])

        S.mark('C')
        S.barrier()
        wout_b = ab(R3 + 8 * KB, 8 * 1024).rearrange("p (k c) -> p k c", c=1024)
        rowS = af(R4, 1024)
        rowB = af(R4 + 4 * KB, 1024)
        h2tmp = af(R4 + 8 * KB, 1024)
        h2tok = [ab(R4 + 12 * KB + i * 2 * KB, 1024) for i in range(2)]
        Mbf = ab(R4 + 16 * KB, 16 * 32).rearrange("p (t e) -> p t e", e=32)
        onesf = af(R4 + 17 * KB, 128)
        utf = af(R4 + 17 * KB + 512, 128)
        utb = ab(R4 + 18 * KB, 128)
        iota32 = af(R4 + 18 * KB + 256, 32)
        diag = [af(R4 + 19 * KB + i * 512, 128) for i in range(2)]
        misc = af(R4 + 20 * KB, 256)
        misc2 = af(R4 + 21 * KB, 256)
        posf_all = af(R4 + 22 * KB, 512).rearrange("p (t e) -> p t e", e=32)
        ef_all = af(R4 + 24 * KB, 64).rearrange("p (t k) -> p t k", k=4)
        rk = af(R4 + 25 * KB, 256)
        hbRow = af(R4 + 26 * KB, 32)
        hcRow = af(R4 + 26 * KB + 128, 32)
        xt2s = [af(R4 + 32 * KB, 1024), af(R4 + 44 * KB, 1024)]
        xn2s = [af(R4 + 36 * KB, 1024), af(R4 + 27 * KB, 1024)]
        h2fs = [af(R4 + 40 * KB, 1024).rearrange("p (k t) -> p k t", t=128), af(R3 + 24 * KB, 1024).rearrange("p (k t) -> p k t", t=128)]
        bdp = af(R3, 1024)
        GTt = af(R3 + 4 * KB, 128)
        wr_f = af(R3 + 5 * KB, 8 * 32).rearrange("p (k c) -> p k c", c=32)
        T_junk3 = ab(R3 + 6 * KB, 1024)
        dma("pool", wout_b, wout_d.rearrange("(k p) c -> p k c", p=128), [], ["wout"], "woutld")
        dma("sp", wr_f, wr_d.rearrange("(k p) c -> p k c", p=128), [], ["wr"], "wrld")
        dma("sp", bdp[0:32, :], bd_d, [], ["bdp"], "bdld")
        for kc in range(8):
            stt(wout_b[:, kc, :], wout_b[:, kc, :], prm[:, P_GCAT + kc:P_GCAT + kc + 1], grow[:, 0, :], ALU.mult, ALU.mult,
                ["wout", "prm", ("grow", 0)], ["wout"])
        tt("dve", bdp[0:32, :], bdp[0:32, :], grow[0:32, 1, :], ALU.mult, ["bdp", ("grow", 1)], ["bdp"])
        bias2 = modc[:, 2, :]
        memset("pool", onesf, 1.0, ["onesf"])
        memset("pool", utf, 1.0, ["utf"])
        S.add("pool", lambda e: e.affine_select(out=utf, in_=utf, pattern=[[1, 128]], compare_op=ALU.is_gt, fill=0.0, base=0,
                                               channel_multiplier=-1), reads=["utf"], writes=["utf"])
        cp("dve", utb, utf, ["utf"], ["utb"])
        S.add("pool", lambda e: e.iota(iota32, pattern=[[1, 32]], base=0, channel_multiplier=0, allow_small_or_imprecise_dtypes=True),
              writes=["iota32"])
        for which, (colv, rowv, ck) in enumerate(((scale2, rowS, "scale2"), (bias2, rowB, "modc2"))):
            for kc in range(8):
                dg = diag[kc % 2]
                ts("dve", dg, identf, colv[:, kc:kc + 1], ALU.mult, ["cst", ck], [("diag", kc % 2)])
                bk = 2 + kc // 4
                mm(bank(bk)[:, (kc % 4) * 128:(kc % 4) * 128 + 128], onesf, dg, True, True, ["onesf", ("diag", kc % 2)], [PSK(bk)])
            for h_ in range(2):
                cp("act", rowv[:, h_ * 512:(h_ + 1) * 512], bank(2 + h_), [PSK(2 + h_)], ["row%d" % which])
        def stageA(t):
                xt2 = xt2s[t % 2]; xn2 = xn2s[t % 2]; h2f = h2fs[t % 2]
                XT = ("xt2", t % 2); XN = ("xn2", t % 2); HF = ("h2f", t % 2)
                dma("sp", xt2, x_d[t * 128:(t + 1) * 128, :], [], [XT], "xt2ld%d" % (t % 2))
                for nb in range(2):
                    for kc in range(8):
                        mm(bank(nb), mixT[:, kc, t * 128:(t + 1) * 128], wout_b[:, kc, nb * 512:(nb + 1) * 512], kc == 0, kc == 7,
                           ["wout"], [PSK(nb)])
                    tt("dve", xres[:, t, nb * 512:(nb + 1) * 512], bank(nb), xt2[:, nb * 512:(nb + 1) * 512], ALU.add, [PSK(nb), XT], [("xres", t)])
                act(T_junk3, xres[:, t, :], AF.Square, [("xres", t)], ["junk3"], accum=ssq_d[:, t:t + 1])
                act(std_d[:, t:t + 1], ssq_d[:, t:t + 1], AF.Sqrt, ["junk3"], ["std_d"], scale=1.0 / D, bias=epsc)
                S.add("dve", lambda e, t=t: e.reciprocal(out=rstd_d[:, t:t + 1], in_=std_d[:, t:t + 1]), reads=["std_d"], writes=["rstd_d"])
                ts("dve", xn2, xres[:, t, :], rstd_d[:, t:t + 1], ALU.mult, [("xres", t), "rstd_d"], [XN])

        def stageB(t):
                xt2 = xt2s[t % 2]; xn2 = xn2s[t % 2]; h2f = h2fs[t % 2]
                XT = ("xt2", t % 2); XN = ("xn2", t % 2); HF = ("h2f", t % 2)
                for kc in range(8):
                    b_ = 2 + kc // 4
                    tr(bank(b_)[:, (kc % 4) * 128:(kc % 4) * 128 + 128], xn2[:, kc * 128:(kc + 1) * 128], identf, [XN, "cst"], [PSK(b_)])
                for kc in range(8):
                    b_ = 2 + kc // 4
                    act(h2f[:, kc, :], bank(b_)[:, (kc % 4) * 128:(kc % 4) * 128 + 128], AF.Identity, [PSK(b_), "scale2", "modc2"], [HF],
                        scale=scale2[:, kc:kc + 1], bias=bias2[:, kc:kc + 1])
                for kc in range(8):
                    mm(bank(4)[:, 0:32], h2f[:, kc, :], wr_f[:, kc, :], kc == 0, kc == 7, [HF, "wr"], [PSK(4)])
                lg = rt[:, 0:32]; top8 = rt[:, 32:40]; msk = rt[:, 40:72] if False else smalls[:, 304:336]
                ex = smalls[:, 336:368]; nmx = smalls[:, 368:369]; ssum = smalls[:, 369:370]; rsum = smalls[:, 370:371]
                tt("dve", lg, bank(4)[:, 0:32], prm[:, P_BROUT:P_BROUT + 32], ALU.add, [PSK(4), "prm"], ["lg"])
                S.add("dve", lambda e, lg=lg, top8=top8: e.max(out=top8, in_=lg), reads=["lg"], writes=["top8"])
                ts("dve", msk, lg, top8[:, 3:4], ALU.is_ge, ["lg", "top8"], ["msk"])
                ts("dve", nmx, top8[:, 0:1], -1.0, ALU.mult, ["top8"], ["nmx"])
                act(ex, lg, AF.Exp, ["lg", "nmx"], ["ex"], bias=nmx)
                tt("dve", ex, ex, msk, ALU.mult, ["ex", "msk"], ["ex"])
                S.add("dve", lambda e, ex=ex, ssum=ssum: e.tensor_reduce(out=ssum, in_=ex, axis=AX.X, op=ALU.add), reads=["ex"], writes=["ssum"])
                S.add("dve", lambda e, ssum=ssum, rsum=rsum: e.reciprocal(out=rsum, in_=ssum), reads=["ssum"], writes=["rsum"])
                ts("dve", Gt[:, t, :], ex, rsum, ALU.mult, ["ex", "rsum"], [("G", t)])
                S.add("dve", lambda e, lg=lg, top8=top8: e.max_index(out=idx8[:], in_max=top8, in_values=lg), reads=["lg", "top8"], writes=["idx8"])
                ex4 = misc[:, 88:92]
                cp("dve", ef_all[:, t, :], idx8[:, 0:4], ["idx8"], [("ef", t)])
                cp("dve", Mbf[:, t, :], msk, ["msk"], [("Mbf", t)])
                for i2 in range(t + 1):
                    mm(bank(4)[:, 64:96], ones_b if i2 < t else utb, Mbf[:, i2, :], i2 == 0, i2 == t,
                       [("Mbf", i2), "cbf1", "utb"], [PSK(4)])
                cp("dve", posf_all[:, t, :], bank(4)[:, 64:96], [PSK(4)], [("posf", t)])
                act(ex4, top8[:, 0:4], AF.Exp, ["top8", "nmx"], ["ex4"], bias=nmx)
                ts("dve", Gk[:, t, :], ex4, rsum, ALU.mult, ["ex4", "rsum"], [("Gk", t)])

        stageA(0)
        for t in range(NT):
            if t + 1 < NT:
                stageA(t + 1)
            stageB(t)
        allM = [("Mbf", i) for i in range(NT)]
        for i in range(NT):
            mm(bank(5)[0:32, 0:2], Mbf[:, i, :], ones_b[:, 0:2], i == 0, i == NT - 1, allM + ["cbf1"], [PSK(5)])
        for i in range(NT):
            mm(bank(5)[:, 64:96], ones_b, Mbf[:, i, :], i == 0, i == NT - 1, allM + ["cbf1"], [PSK(5)])
        cntc = rk[0:32, 0:1]; cntr = rk[0:32, 32:64]; gt_ = rk[0:32, 64:96]; eq_ = rk[0:32, 96:128]; low_ = rk[0:32, 128:160]
        rankc = rk[0:32, 160:161]; Rm = rk[0:32, 168:168 + HOTN]; tmp8 = rk[0:32, 176:176 + HOTN]; hbc = rk[0:32, 184:185]; hcc = rk[0:32, 185:186]
        ish = rk[0:32, 186:187]; dg32 = rk[0:32, 192:224]
        pidx = cst[0:32, C_PIDX:C_PIDX + 1]
        cp("dve", cntc, bank(5)[0:32, 0:1], [PSK(5)], ["rk"])
        cp("dve", cntr, bank(5)[0:32, 64:96], [PSK(5), "rk"], ["rk"])
        ts("dve", gt_, cntr, cntc, ALU.is_gt, ["rk"], ["rk"])
        ts("dve", eq_, cntr, cntc, ALU.is_equal, ["rk"], ["rk"])
        ts("dve", low_, iota32[0:32, :], pidx, ALU.is_lt, ["iota32", "cst", "rk"], ["rk"])
        tt("dve", eq_, eq_, low_, ALU.mult, ["rk"], ["rk"])
        tt("dve", gt_, gt_, eq_, ALU.add, ["rk"], ["rk"])
        S.add("dve", lambda e: e.tensor_reduce(out=rankc, in_=gt_, axis=AX.X, op=ALU.add), reads=["rk"], writes=["rk"])
        ts("dve", Rm, iota32[0:32, 0:HOTN], rankc, ALU.is_equal, ["rk", "iota32"], ["rk"])
        mm(bank(5)[0:1, 128:128 + HOTN], pidx, Rm, True, True, ["rk", "cst"], [PSK(5)])
        cp("dve", pi_i[0:1, 0:HOTN], bank(5)[0:1, 128:128 + HOTN], [PSK(5)], ["pi_i"])
        tt("dve", tmp8, Rm, cst[0:32, C_HB:C_HB + HOTN], ALU.mult, ["rk", "cst"], ["rk"])
        S.add("dve", lambda e: e.tensor_reduce(out=hbc, in_=tmp8, axis=AX.X, op=ALU.add), reads=["rk"], writes=["rk"])
        S.add("dve", lambda e: e.tensor_reduce(out=ish, in_=Rm, axis=AX.X, op=ALU.add), reads=["rk"], writes=["rk"])
        ts("dve", ish, ish, -1.0e6, ALU.mult, ["rk"], ["rk"], s2=1.0e6, op1=ALU.add)
        tt("dve", hbc, hbc, ish, ALU.add, ["rk"], ["rk"])
        tt("dve", tmp8, Rm, cst[0:32, C_HC:C_HC + HOTN], ALU.mult, ["rk", "cst"], ["rk"])
        S.add("dve", lambda e: e.tensor_reduce(out=hcc, in_=tmp8, axis=AX.X, op=ALU.add), reads=["rk"], writes=["rk"])
        for colv, rowv, c0 in ((hbc, hbRow, 256), (hcc, hcRow, 320)):
            ts("dve", dg32, identf[0:32, 0:32], colv, ALU.mult, ["rk", "cst"], ["rk"])
            mm(bank(5)[:, c0:c0 + 32], onesf[0:32, :], dg32, True, True, ["rk", "onesf"], [PSK(5)])
            cp("dve", rowv, bank(5)[:, c0:c0 + 32], [PSK(5)], ["hrows"])
        vecR = af(R3 + 28 * KB, 1024)
        pkA = vecR[:, 0:64]; hbA = vecR[:, 64:128]; hcA = vecR[:, 128:192]
        for t in range(NT):
            ef = ef_all[:, t, :]
            oh4 = misc2[:, 0:128].rearrange("p (k e) -> p k e", e=32)
            pr4 = misc2[:, 128:256].rearrange("p (k e) -> p k e", e=32)
            tt("dve", oh4, iota32.unsqueeze(1).broadcast_to([128, 4, 32]), ef.unsqueeze(2).broadcast_to([128, 4, 32]), ALU.is_equal,
               ["iota32", ("ef", t)], ["oh4"])
            for src, dstA, sk_ in ((posf_all[:, t, :], pkA, ("posf", t)), (hbRow, hbA, "hrows"), (hcRow, hcA, "hrows")):
                tt("dve", pr4, oh4, src.unsqueeze(1).broadcast_to([128, 4, 32]), ALU.mult, ["oh4", sk_], ["pr4"])
                S.add("dve", lambda e, dstA=dstA, pr4=pr4, t=t: e.tensor_reduce(out=dstA[:, 4 * t:4 * t + 4], in_=pr4, axis=AX.X, op=ALU.add),
                      reads=["pr4"], writes=["pkv"])
        efA = ef_all.rearrange("p t k -> p (t k)")
        allE = [("ef", t) for t in range(NT)]
        allG = [("Gk", t) for t in range(NT)]
        instat, fs, q_, inh1, inh2, fh, flat, valid, ovf = [vecR[:, 192 + i * 64:256 + i * 64] for i in range(9)]
        V = ["pkv", "vec"]
        ts("dve", instat, pkA, float(CAP), ALU.is_lt, V, ["vec"])
        stt(fs, efA, float(CAP), pkA, ALU.mult, ALU.add, V + allE, ["vec"])
        ts("dve", q_, pkA, -float(CAP), ALU.add, V, ["vec"])
        ts("dve", inh1, q_, 0.0, ALU.is_ge, V, ["vec"])
        tt("dve", inh2, q_, hcA, ALU.is_lt, V, ["vec"])
        tt("dve", inh1, inh1, inh2, ALU.mult, V, ["vec"])
        tt("dve", fh, hbA, q_, ALU.add, V, ["vec"])
        ts("dve", fh, fh, float(HOT0), ALU.add, V, ["vec"])
        tt("dve", fs, fs, instat, ALU.mult, V, ["vec"])
        tt("dve", fh, fh, inh1, ALU.mult, V, ["vec"])
        tt("dve", flat, fs, fh, ALU.add, V, ["vec"])
        tt("dve", valid, instat, inh1, ALU.add, V, ["vec"])
        ts("dve", ovf, valid, -1.0e6, ALU.mult, V, ["vec"], s2=1.0e6, op1=ALU.add)
        tt("dve", flat, flat, ovf, ALU.add, V, ["vec"])
        cp("dve", IDX[:, :], flat, V, ["IDXall"])
        GkA = Gk[:].rearrange("p t k -> p (t k)")
        tt("dve", GkA, GkA, valid, ALU.mult, V + allG, allG)
        for t in range(NT):
            hb = h2tok[t % 2]
            xn2 = xn2s[t % 2]
            XN = ("xn2", t % 2)
            act(xn2, xres[:, t, :], AF.Identity, [("xres", t), "rstd_d"], [XN], scale=rstd_d[:, t:t + 1])
            tt("dve", h2tmp, xn2, rowS, ALU.mult, [XN, "row0"], ["h2tmp"])
            tt("pool", hb, h2tmp, rowB, ALU.add, ["h2tmp", "row1"], [("h2tok", t % 2)])
            for k in range(4):
                S.add("pool", lambda e, t=t, k=k, hb=hb: e.indirect_dma_start(
                    out=Hs, out_offset=bass.IndirectOffsetOnAxis(ap=IDX[:, 4 * t + k:4 * t + k + 1], axis=0), in_=hb, in_offset=None,
                    bounds_check=bnd(e), oob_is_err=False),
                    reads=[("h2tok", t % 2), "IDXall"], writes=[("Hs", t, k)], semkey="hs_sc%d_%d" % (t % 2, k))
            tr(bank(6)[0:32, 0:128], Gt[:, t, :], identf, [("G", t), "cst"], [PSK(6)])
            cp("act", GTt[0:32, :], bank(6)[0:32, 0:128], [PSK(6)], ["GTt"])
            for nb in range(2):
                mm(bank(nb), GTt[0:32, :], bdp[0:32, nb * 512:(nb + 1) * 512], True, True, ["GTt", "bdp"], [PSK(nb)])
                tt("dve", xres[:, t, nb * 512:(nb + 1) * 512], xres[:, t, nb * 512:(nb + 1) * 512], bank(nb), ALU.add,
                   [PSK(nb), ("xres", t), XN], [("xres", t)])
        dump("d_x1", xres.rearrange("p t d -> p (t d)"), [("xres", t) for t in range(NT)])
        dump("d_G", Gt[:].rearrange("p t e -> p (t e)"), [("G", t) for t in range(NT)])
        dump("d_gk", Gk[:].rearrange("p t k -> p (t k)"), [("Gk", t) for t in range(NT)])

        S.mark('D')
        S.barrier()
        RE = R2
        MAXS = 1024
        hs_tok = [ab(RE + i * 8 * KB, 4 * 1024).rearrange("p (s d) -> p s d", d=1024) for i in range(2)]
        h2g = [ab(RE + 16 * KB + i * 16 * KB, 8 * MAXS).rearrange("p (k s) -> p k s", s=MAXS) for i in range(2)]
        actE = ab(RE + 48 * KB, 8 * MAXS).rearrange("p (k s) -> p k s", s=MAXS)
        slabE = [ab(RE + 64 * KB + i * 4 * KB, 8 * 256).rearrange("p (k c) -> p k c", c=256) for i in range(3)]
        wdE = [ab(RE + 76 * KB + i * 16 * KB, 8 * 1024).rearrange("p (k c) -> p k c", c=1024) for i in range(2)]
        ytile = [af(RE + 108 * KB + i * 4 * KB, 1024) for i in range(2)]
        gcb = [af(RE + 116 * KB + i * 2 * KB, 512) for i in range(2)]
        u1b = [af(RE + 120 * KB + i * 2 * KB, 512) for i in range(2)]
        gsb = [ab(RE + 124 * KB + i * KB, 512) for i in range(2)]
        ykb = [af(RE + i * 4 * KB, 1024) for i in range(12)]
        bu1 = prm[:, P_BU:P_BU + 256]
        ts("dve", bu1, bu1, 1.0, ALU.add, ["prm"], ["prm"])
        allHs = [("Hs", t, k) for t in range(NT) for k in range(4)]
        allYs = []
        cnt = {"sc": 0, "hl": 0, "tb": 0, "ew": 0, "yb": 0}
        g2row_ = grow[:, 1, :]
        ak = "actE"
        vals = {}

        def chunks(nt):
            out, t0 = [], 0
            while t0 < nt:
                n = min(4, nt - t0)
                out.append((t0, n))
                t0 += n
            return out

        def gather(pi_, row0, nt):
            hg = h2g[pi_ % 2]
            for (t0, n) in chunks(nt):
                hi = cnt["hl"] % 2
                cnt["hl"] += 1
                hb = hs_tok[hi]
                hk = ("hs_tok", hi)
                r0 = row0 + t0 * 128
                dma("sp", hb[:, 0:n, :], Hs[r0:r0 + n * 128, :].rearrange("(s p) d -> p s d", p=128), allHs, [hk], "hsld%d" % hi)
                for st_ in range(n):
                    bk = cnt["tb"] % 2
                    cnt["tb"] += 1
                    for kc in range(8):
                        tr(bankb(bk)[:, kc * 128:(kc + 1) * 128], hb[:, st_, kc * 128:(kc + 1) * 128], ident_b, [hk, "cbf"], [PSK(bk)])
                    c0 = (t0 + st_) * 128
                    cp("act", hg[:, :, c0:c0 + 128], bankb(bk)[:, 0:1024].rearrange("p (k s) -> p k s", s=128), [PSK(bk)], [("h2g", pi_ % 2)])

        def gate_up(pi_, nt, slab_dma, bgcol, bucol, bkeys):
            hg = h2g[pi_ % 2]
            gk_ = ("h2g", pi_ % 2)
            for j in range(8):
                sl = slabE[cnt["sc"] % 3]
                sk = ("slab", cnt["sc"] % 3)
                slab_dma(j, sl, sk, "slabld%d" % (cnt["sc"] % 3))
                cnt["sc"] += 1
                for (t0, n) in chunks(nt):
                    hsl = slice(t0 * 128, (t0 + n) * 128)
                    w_ = n * 128
                    i2 = cnt["ew"] % 2
                    cnt["ew"] += 1
                    pg, pu = 2 + i2 * 2, 3 + i2 * 2
                    for kc in range(8):
                        mm(bank(pg)[:, 0:w_], sl[:, kc, 0:128], hg[:, kc, hsl], kc == 0, kc == 7, [sk, gk_], [PSK(pg)])
                    for kc in range(8):
                        mm(bank(pu)[:, 0:w_], sl[:, kc, 128:256], hg[:, kc, hsl], kc == 0, kc == 7, [sk, gk_], [PSK(pu)])
                    ts("dve", gcb[i2][:, 0:w_], bank(pg)[:, 0:w_], bgcol(j), ALU.add, [PSK(pg)] + bkeys, [("gc", i2)], s2=7.0, op1=ALU.min)
                    act(gsb[i2][:, 0:w_], gcb[i2][:, 0:w_], AF.Gelu_apprx_sigmoid, [("gc", i2)], [("gs", i2)])
                    ts("dve", u1b[i2][:, 0:w_], bank(pu)[:, 0:w_], bucol(j), ALU.add, [PSK(pu)] + bkeys, [("u1", i2)], s2=8.0, op1=ALU.min)
                    stt(actE[:, j, hsl], u1b[i2][:, 0:w_], -6.0, gsb[i2][:, 0:w_], ALU.max, ALU.mult, [("u1", i2), ("gs", i2)], [ak])

        def down(pi_, row0, nt):
            wb = wdE[pi_ % 2]
            wk = ("wd", pi_ % 2)
            for st_ in range(nt):
                yb = cnt["yb"] % 2
                cnt["yb"] += 1
                yt_ = ytile[yb]
                for nb in range(2):
                    pd = 6 + nb
                    for kc in range(8):
                        mm(bank(pd), actE[:, kc, st_ * 128:(st_ + 1) * 128], wb[:, kc, nb * 512:(nb + 1) * 512], kc == 0, kc == 7,
                           [ak, wk], [PSK(pd)])
                    cp("act" if nb == 0 else "dve", yt_[:, nb * 512:(nb + 1) * 512], bank(pd), [PSK(pd)], [("ytile", yb)])
                r0 = row0 + st_ * 128
                dma("sp", Ys[r0:r0 + 128, :], yt_, [("ytile", yb)], [("Ys", r0)], "ys_st%d" % yb)
                allYs.append(("Ys", r0))

        def wd_scale(pi_):
            wb = wdE[pi_ % 2]
            wk = ("wd", pi_ % 2)
            for kc in range(8):
                tt("pool", wb[:, kc, :], wb[:, kc, :], g2row_, ALU.mult, [wk, ("grow", 1)], [wk])

        passes = [(e_ * CAP, CAP // 128, "s", e_) for e_ in range(nexp)]
        if nexp == NE:
            hb0 = 0
            for r_ in range(HOTN):
                passes.append((HOT0 + hb0 * 128, HOT_T[r_], "h", r_))
                hb0 += HOT_T[r_]

        def prep_pass(pi_):
            row0, nt, kind, id_ = passes[pi_]
            wb = wdE[pi_ % 2]
            wk = ("wd", pi_ % 2)
            if kind == "s":
                getrow = lambda id_=id_: wall_d[id_:id_ + 1, :]
                bgcol = lambda j, id_=id_: prm[:, P_BG + id_ * 8 + j:P_BG + id_ * 8 + j + 1]
                bucol = lambda j, id_=id_: bu1[:, id_ * 8 + j:id_ * 8 + j + 1]
                bkeys = ["prm"]
            else:
                r_ = id_

                def vload(e, r_=r_):
                    vals[r_] = e.value_load(pi_i[0:1, r_:r_ + 1])
                    return None
                S.add("pool", vload, reads=["pi_i"])
                getrow = lambda r_=r_: wall_d[bass.ds(vals[r_], 1), :]
                hbk_ = ("hbias", r_ % 2)
                S.add("pool", lambda e, r_=r_, getrow=getrow: e.dma_start(
                    out=hbias[:, r_ % 2, :], in_=getrow()[:, WOFF_B:WOFF_B + 2048].rearrange("e (p c) -> p (e c)", c=16)),
                    reads=[], writes=[hbk_], semkey="hbld%d" % (r_ % 2))
                ts("dve", hbias[:, r_ % 2, 8:16], hbias[:, r_ % 2, 8:16], 1.0, ALU.add, [hbk_], [hbk_])
                bgcol = lambda j, r_=r_: hbias[:, r_ % 2, j:j + 1]
                bucol = lambda j, r_=r_: hbias[:, r_ % 2, 8 + j:9 + j]
                bkeys = [hbk_]
            S.add("pool", lambda e, wb=wb, getrow=getrow: e.dma_start(
                out=wb, in_=getrow()[:, WOFF_D:WOFF_D + D * D].rearrange("e (k p c) -> p (e k) c", p=128, c=D)),
                reads=[], writes=[wk], semkey="wdld%d" % (pi_ % 2))

            def slab_dma(j, sl, sk, sem, getrow=getrow):
                S.add("pool", lambda e, j=j, sl=sl, getrow=getrow: e.dma_start(
                    out=sl, in_=getrow()[:, j * D * 256:(j + 1) * D * 256].rearrange("e (k p c) -> p (e k) c", p=128, c=256)),
                    reads=[], writes=[sk], semkey=sem)
            return (slab_dma, bgcol, bucol, bkeys)

        if passes:
            gather(0, passes[0][0], passes[0][1])
        for pi_ in range(len(passes)):
            row0, nt, kind, id_ = passes[pi_]
            src = prep_pass(pi_)
            gate_up(pi_, nt, *src)
            wd_scale(pi_)
            if pi_ + 1 < len(passes):
                gather(pi_ + 1, passes[pi_ + 1][0], passes[pi_ + 1][1])
            down(pi_, row0, nt)
        NYK = 12
        for b_ in range(NYK):
            memset("pool", ykb[b_], 0.0, [("yk", b_), ("hs_tok", b_ // 2) if b_ < 4 else ("h2g", (b_ - 4) // 4)])
        cnt_ = 0
        for t in range(NT):
            for k in range(4):
                b_ = cnt_ % NYK
                cnt_ += 1
                S.add("pool", lambda e, t=t, k=k, b_=b_: e.indirect_dma_start(
                    out=ykb[b_], out_offset=None, in_=Ys, in_offset=bass.IndirectOffsetOnAxis(ap=IDX[:, 4 * t + k:4 * t + k + 1], axis=0),
                    bounds_check=bnd(e), oob_is_err=False),
                    reads=allYs + ["IDXall"], writes=[("yk", b_)], semkey="ykld%d" % b_)
                stt(xres[:, t, :], ykb[b_], Gk[:, t, k:k + 1], xres[:, t, :], ALU.mult, ALU.add,
                    [("yk", b_), ("Gk", t), ("xres", t)], [("xres", t)])
        S.disabled = False
        for t in range(NT):
            dma("sp", out_d[t * 128:(t + 1) * 128, :], xres[:, t, :], [("xres", t)], [("out", t)], "outst%d" % (t % 4))
        S.add("sp", None, reads=[("out", t) for t in range(NT)] + (["dbg_" + n for n in dbg] if debug else []))
        S.emit(nc, st)
    return nc


_CACHE = {}


def _prep(inp, b):
    f = lambda a: np.ascontiguousarray(a, dtype=np.float32)
    col = lambda v: f(np.asarray(v).reshape(-1, 128).T)
    prm = np.zeros((128, NPRM), np.float32)
    prm[:, P_CCOL:P_CCOL + 8] = col(inp["c"][b])
    prm[:, P_BADA:P_BADA + 48] = col(inp["b_ada"][0])
    prm[:, P_NMIX:P_NMIX + 8] = col(inp["norm_mix"][0])
    prm[:, P_NFFN:P_NFFN + 8] = col(inp["norm_ffn"][0])
    perm = []
    for i in range(4):
        perm += list(range(64 * i, 64 * i + 64)) + list(range(256 + 64 * i, 256 + 64 * i + 64))
    perm += list(range(512, 640)) + list(range(768, 1280)) + list(range(640, 768))
    perm = np.array(perm)
    b_in = inp["b_in"][0][perm]
    prm[:, P_BIN:P_BIN + 9] = col(b_in[:1152])
    prm[:, P_QG] = np.tile(inp["q_norm"][0], 2)
    prm[:, P_QG + 1] = np.tile(inp["k_norm"][0], 2)
    prm[:, P_LAMR:P_LAMR + 32] = np.tile(inp["lam_re"][0].T, (2, 1))
    prm[:, P_LAMI:P_LAMI + 32] = np.tile(inp["lam_im"][0].T, (2, 1))
    prm[:, P_LDT:P_LDT + 32] = np.tile(inp["log_dt"][0][None, :], (128, 1))
    prm[:, P_DSK:P_DSK + 4] = col(inp["d_skip"][0])
    prm[:, P_BGLU:P_BGLU + 4] = col(inp["b_glu"][0])
    prm[:, P_GCAT:P_GCAT + 8] = col(np.concatenate([inp["attn_out_norm"][0], inp["ssm_out_norm"][0]]))
    prm[:, P_SINK:P_SINK + 8] = np.tile(inp["sinks"][0][None, :], (128, 1))
    prm[:, P_BROUT:P_BROUT + 32] = np.tile(inp["b_router"][0][None, :], (128, 1))
    prm[:, P_BVROW:P_BVROW + 128] = np.tile(b_in[1152:1280][None, :], (128, 1))
    bgu = inp["b_gate_up"][0]
    prm[:, P_BG:P_BG + 256] = bgu[:, 0::2].reshape(32, 8, 128).transpose(2, 0, 1).reshape(128, 256)
    prm[:, P_BU:P_BU + 256] = bgu[:, 1::2].reshape(32, 8, 128).transpose(2, 0, 1).reshape(128, 256)
    return prm, perm


def _shared(inp):
    f = lambda a: np.ascontiguousarray(a, dtype=np.float32)
    perm = None
    _, perm = _prep(inp, 0)
    sh = {}
    sh["cst"] = _consts()
    sh["w_ada"] = f(inp["w_ada"][0])
    bada = inp["b_ada"][0]
    sh["bada_g"] = f(np.tile(np.concatenate([bada[2048:3072], bada[5120:6144]])[None, :], (128, 1)))
    sh["w_in"] = f(inp["w_in"][0][:, perm])
    bre = inp["b_re"][0].transpose(1, 0, 2).reshape(64, 512)
    bim = inp["b_im"][0].transpose(1, 0, 2).reshape(64, 512)
    sh["ssm_sb"] = f(np.stack([np.concatenate([bre, bim], 0), np.concatenate([bim, bre], 0)], axis=1))
    cre = inp["c_re"][0].transpose(2, 0, 1).reshape(64, 512)
    cim = inp["c_im"][0].transpose(2, 0, 1).reshape(64, 512)
    sh["ssm_sc"] = f(np.stack([np.concatenate([cre, cim], 0), np.concatenate([cim, cre], 0)], axis=1))
    sh["w_glu"] = f(inp["w_glu"][0])
    sh["w_out"] = f(inp["w_out"][0])
    sh["w_router"] = f(inp["w_router"][0])
    wgu = inp["w_gate_up"][0]
    g = wgu[:, :, 0::2].reshape(32, 1024, 8, 128)
    u = wgu[:, :, 1::2].reshape(32, 1024, 8, 128)
    w_gu = np.concatenate([g, u], axis=3).transpose(0, 2, 1, 3)
    bgu = inp["b_gate_up"][0]
    bg_ = bgu[:, 0::2].reshape(32, 8, 128).transpose(0, 2, 1)
    bu_ = bgu[:, 1::2].reshape(32, 8, 128).transpose(0, 2, 1)
    b_gu = np.concatenate([bg_, bu_], axis=2)
    sh["wall"] = f(np.concatenate([w_gu.reshape(32, -1), inp["w_down"][0].reshape(32, -1), b_gu.reshape(32, -1)], axis=1))
    sh["b_down"] = f(inp["b_down"][0])
    return sh


def kernel(**inputs):
    inp = {k: np.asarray(v) for k, v in inputs.items()}
    debug = bool(int(os.environ.get("MK_DEBUG", "0")))
    nexp = int(os.environ.get("MK_NEXP", str(NE)))
    cores = [int(c) for c in os.environ.get("MK_CORES", "0,1,2,3,4,5,6,7").split(",")]
    key = (debug, nexp, os.environ.get('MK_STOP', ''))
    if key not in _CACHE:
        _CACHE[key] = build(debug=debug, nexp=nexp)
    nc = _CACHE[key]
    sh = _shared(inp)
    in_maps = []
    for b in cores:
        m = dict(sh)
        m["x"] = np.ascontiguousarray(inp["x"][b], dtype=np.float32)
        m["prm"], _ = _prep(inp, b)
        in_maps.append(m)
    res = run_bass_kernel_spmd(nc, in_maps, core_ids=list(range(len(cores))))
    if debug:
        kernel.last = res.results
    out = np.zeros((8, S_, D), np.float32)
    for i, b in enumerate(cores):
        out[b] = res.results[i]["out"]
    return out
```

```python
import os
import math
import numpy as np
from contextlib import ExitStack
import concourse.bass as bass
import concourse.mybir as mybir
from concourse.bass_utils import run_bass_kernel_spmd

F32 = mybir.dt.float32
BF16 = mybir.dt.bfloat16
AF = mybir.ActivationFunctionType
ALU = mybir.AluOpType
AX = mybir.AxisListType

S_ = 2048
D = 1024
NT = 16
NE = 32
CAP = 768
HOT_T = [4, 4]
HOTN = len(HOT_T)
HOT0 = NE * CAP
NROWS = HOT0 + 128 * sum(HOT_T)

EPS = 1e-6
KB = 1024
MAGIC = 12582912.0
TWO_PI = 2.0 * math.pi


class _Op:
    __slots__ = ("idx", "eng", "fn", "semkey", "deps", "sig", "group")


class Sched:
    ENGS = ("pe", "act", "dve", "pool", "sp")

    def __init__(self):
        self.ops = []
        self.lastw = {}
        self.readers = {}
        self.barrier_deps = set()
        self.last_on_eng = {}
        self.last_dma = {}

    def add(self, eng, fn, reads=(), writes=(), semkey=None, group=False):
        if getattr(self, 'disabled', False):
            return None
        op = _Op()
        op.idx = len(self.ops)
        op.eng = eng
        op.fn = fn
        op.semkey = semkey
        op.group = group
        op.sig = None
        deps = set(self.barrier_deps)
        for k in reads:
            w = self.lastw.get(k)
            if w is not None:
                deps.add(w)
        for k in writes:
            w = self.lastw.get(k)
            if w is not None:
                deps.add(w)
            deps.update(self.readers.get(k, {}).values())
        if eng == "pe" and semkey is None:
            deps = {d for d in deps if not (self.ops[d].eng == "pe" and self.ops[d].semkey is None)}
        op.deps = deps
        for k in reads:
            r = self.readers.setdefault(k, {})
            rk = eng if semkey is None else ("dma", op.idx)
            r[rk] = op.idx
        for k in writes:
            self.lastw[k] = op.idx
            self.readers[k] = {}
        self.ops.append(op)
        if semkey is None:
            self.last_on_eng[eng] = op.idx
        else:
            self.last_dma[semkey] = op.idx
        return op

    def mark(self, name):
        if os.environ.get('MK_STOP', '') == name:
            self.disabled = True

    def barrier(self):
        self.barrier_deps = set(self.last_on_eng.values()) | set(self.last_dma.values())

    def emit(self, nc, stack):
        needed = set()
        for op in self.ops:
            needed |= op.deps
        sems = {}

        def getsem(name):
            if name not in sems:
                sems[name] = stack.enter_context(nc.semaphore("s_%d" % len(sems)))
            return sems[name]

        cnt = {}
        for op in self.ops:
            if op.semkey is not None:
                cnt[op.semkey] = cnt.get(op.semkey, 0) + 16
                op.sig = (op.semkey, cnt[op.semkey])
            elif op.idx in needed:
                cnt[op.eng] = cnt.get(op.eng, 0) + 1
                op.sig = (op.eng, cnt[op.eng])
        for op in self.ops:
            if op.semkey is not None and op.group:
                op.sig = (op.semkey, cnt[op.semkey])
        per_eng = {e: [] for e in self.ENGS}
        seen = {e: {} for e in self.ENGS}
        for op in self.ops:
            w = {}
            for d in op.deps:
                name, val = self.ops[d].sig
                if val > w.get(name, 0):
                    w[name] = val
            waits = []
            for name, val in w.items():
                if seen[op.eng].get(name, 0) < val:
                    seen[op.eng][name] = val
                    waits.append((name, val))
            per_eng[op.eng].append((op, waits))
        handles = {"pe": "tensor", "act": "scalar", "dve": "vector", "pool": "gpsimd", "sp": "sync"}
        for op in self.ops:
            if op.sig is not None:
                getsem(op.sig[0])
        block = stack.enter_context(nc.Block())

        def body(engname):
            def _f(eng):
                for op, waits in per_eng[engname]:
                    for name, val in waits:
                        eng.wait_ge(sems[name], val)
                    inst = op.fn(eng) if op.fn is not None else None
                    if op.sig is not None:
                        assert inst is not None
                        if op.semkey is not None:
                            inst.then_inc(sems[op.semkey], 16)
                        else:
                            inst.then_inc(sems[op.eng], 1)
            return _f

        for engname in self.ENGS:
            if per_eng[engname]:
                getattr(block, handles[engname])(body(engname))
        self.counts = cnt


C_IDENT = 0
C_SHIFT = 128
C_BONES = 256
C_NMCUR = 384
C_NMPRV = 512
C_ROWM = 640
C_SGN = 642
C_EPS = 643
C_ONE = 644
C_NHALFPI = 645
C_ROWM4 = 648
C_BD16 = 652
C_HB = 780
C_HC = 788
C_PIDX = 796
NCST = 800


def _consts():
    c = np.zeros((128, NCST), np.float32)
    c[:, C_IDENT:C_IDENT + 128] = np.eye(128)
    sh = np.zeros((128, 128), np.float32)
    for k in range(128):
        sh[k, (k + 64) % 128] = 1.0
    c[:, C_SHIFT:C_SHIFT + 128] = sh
    bo = np.zeros((128, 128), np.float32)
    bo[:64, :64] = 1.0
    bo[64:, 64:] = 1.0
    c[:, C_BONES:C_BONES + 128] = bo
    s = np.arange(128)[:, None]
    t = np.arange(128)[None, :]
    c[:, C_NMCUR:C_NMCUR + 128] = np.where(s <= t, 0.0, -30000.0)
    c[:, C_NMPRV:C_NMPRV + 128] = np.where(s > t, 0.0, -30000.0)
    p = np.arange(128)
    c[:, C_ROWM] = ((p // 16) % 2 == 0)
    c[:, C_ROWM + 1] = ((p // 16) % 2 == 1)
    c[:64, C_SGN] = 1.0
    c[64:, C_SGN] = -1.0
    c[:, C_EPS] = EPS
    c[:, C_ONE] = 1.0
    c[:, C_NHALFPI] = 0.5 * math.pi
    for a4 in range(4):
        c[:, C_ROWM4 + a4] = ((p // 16) % 4 == a4)
    c[:, C_BD16:C_BD16 + 128] = (p[:, None] // 16 == p[None, :] // 16)
    c[:, C_HB:C_HB + HOTN] = (128 * (np.cumsum([0] + HOT_T)[:HOTN]))[None, :]
    c[:, C_HC:C_HC + HOTN] = (128 * np.array(HOT_T))[None, :]
    c[:, C_PIDX] = p
    return c


P_CCOL = 0
P_BADA = 8
P_NMIX = 56
P_NFFN = 64
P_BIN = 72
P_QG = 81
P_LAMR = 83
P_LAMI = 115
P_LDT = 147
P_DSK = 179
P_BGLU = 183
P_GCAT = 187
P_SINK = 195
P_BROUT = 203
P_BVROW = 235
P_BG = 363
P_BU = 619
NPRM = 875


def build(debug=False, nexp=NE):
    nc = bass.Bass("TRN2", target_bir_lowering=False)
    dr = lambda n, s: nc.dram_tensor(n, s, F32, kind="ExternalInput").ap()
    x_d = dr("x", [S_, D])
    cst_d = dr("cst", [128, NCST])
    prm_d = dr("prm", [128, NPRM])
    wada_d = dr("w_ada", [D, 6 * D])
    badag_d = dr("bada_g", [128, 2 * D])
    win_d = dr("w_in", [D, 1280])
    sb_d = dr("ssm_sb", [128, 2, 512])
    sc_d = dr("ssm_sc", [128, 2, 512])
    wglu_d = dr("w_glu", [512, 512])
    wout_d = dr("w_out", [D, D])
    wr_d = dr("w_router", [D, NE])
    WOFF_D = 8 * D * 256
    WOFF_B = WOFF_D + D * D
    WALL = WOFF_B + 128 * 16
    wall_d = dr("wall", [NE, WALL])
    bd_d = dr("b_down", [NE, D])
    out_d = nc.dram_tensor("out", [S_, D], F32, kind="ExternalOutput").ap()
    Hs = nc.dram_tensor("hs_scr", [NROWS, D], BF16, kind="Internal").ap()
    Ys = nc.dram_tensor("ys_scr", [NROWS, D], BF16, kind="Internal").ap()
    dbg = {}
    if debug:
        for n, shp in [("d_hT", [128, 8 * 2048]), ("d_qT", [128, 4 * 2048]), ("d_kT", [128, 2048]), ("d_uT", [128, 4 * 2048]),
                       ("d_v1", [128, 16 * 132]), ("d_mix", [128, 8 * 2048]), ("d_x32", [128, 32 * 257]), ("d_gT", [128, 4 * 2048]),
                       ("d_x1", [128, 16 * 1024]), ("d_G", [128, 16 * 32]), ("d_gk", [128, 64]), ("d_idx", [128, 64]), ("d_mod", [128, 64]),
                       ("d_g1", [128, 2048]), ("d_sp", [128, 9 * 32])]:
            dbg[n] = nc.dram_tensor(n, shp, F32, kind="ExternalOutput").ap()

    S = Sched()
    st = ExitStack()
    with st:
        ARW = 190 * KB // 4
        arena = st.enter_context(nc.sbuf_tensor("arena", [128, ARW], F32))
        cst = st.enter_context(nc.sbuf_tensor("cstt", [128, NCST], F32))
        prm = st.enter_context(nc.sbuf_tensor("prmt", [128, NPRM], F32))
        cbf = st.enter_context(nc.sbuf_tensor("cbf", [128, 5 * 128], BF16))
        smalls = st.enter_context(nc.sbuf_tensor("smalls", [128, 512], F32))
        Gt = st.enter_context(nc.sbuf_tensor("Gt", [128, NT, NE], F32))
        IDX = st.enter_context(nc.sbuf_tensor("idxt", [128, NT * 4], mybir.dt.int32))
        Gk = st.enter_context(nc.sbuf_tensor("gkt", [128, NT, 4], F32))
        idx8 = st.enter_context(nc.sbuf_tensor("idx8", [128, 8], mybir.dt.uint32))
        pi_i = st.enter_context(nc.sbuf_tensor("pi_i", [1, 8], mybir.dt.int32))
        hbias = st.enter_context(nc.sbuf_tensor("hbias", [128, 2, 16], F32))
        psum = st.enter_context(nc.psum_tensor("psum", [128, 4096], F32))

        def af(off, n):
            assert off % 4 == 0
            return arena[:, off // 4: off // 4 + n]

        def ab(off, n):
            assert off % 4 == 0 and n % 2 == 0
            return arena[:, off // 4: off // 4 + n // 2].bitcast(BF16)

        def bank(i):
            return psum[:, i * 512:(i + 1) * 512]

        def bankb(i):
            return bank(i).bitcast(BF16)

        PSK = lambda i: ("ps", i)
        ident_b = cbf[:, 0:128]
        shift_b = cbf[:, 128:256]
        bones_b = cbf[:, 256:384]
        ones_b = cbf[:, 512:640]
        identf = cst[:, C_IDENT:C_IDENT + 128]
        shiftf = cst[:, C_SHIFT:C_SHIFT + 128]
        epsc = cst[:, C_EPS:C_EPS + 1]
        sgn = cst[:, C_SGN:C_SGN + 1]

        def act(out, in_, func, r, w, scale=None, bias=None, accum=None):
            kw = {}
            if scale is not None:
                kw["scale"] = scale
            if bias is not None:
                kw["bias"] = bias
            if accum is not None:
                kw["accum_out"] = accum
            S.add("act", lambda e: e.activation(out=out, in_=in_, func=func, **kw), reads=r, writes=w)

        def tt(eng, out, in0, in1, op, r, w):
            S.add(eng, lambda e: e.tensor_tensor(out=out, in0=in0, in1=in1, op=op), reads=r, writes=w)

        def ts(eng, out, in0, s1, op0, r, w, s2=None, op1=None):
            if op1 is None:
                S.add(eng, lambda e: e.tensor_scalar(out=out, in0=in0, scalar1=s1, scalar2=None, op0=op0), reads=r, writes=w)
            else:
                S.add(eng, lambda e: e.tensor_scalar(out=out, in0=in0, scalar1=s1, scalar2=s2, op0=op0, op1=op1), reads=r, writes=w)

        def stt(out, in0, scalar, in1, op0, op1, r, w):
            S.add("dve", lambda e: e.scalar_tensor_tensor(out=out, in0=in0, scalar=scalar, in1=in1, op0=op0, op1=op1), reads=r, writes=w)

        def cp(eng, out, in_, r, w):
            if eng == "act":
                S.add(eng, lambda e: e.activation(out=out, in_=in_, func=AF.Identity), reads=r, writes=w)
            else:
                S.add(eng, lambda e: e.tensor_copy(out=out, in_=in_), reads=r, writes=w)

        def mm(out, lhsT, rhs, start, stop, r, w):
            S.add("pe", lambda e: e.matmul(out, lhsT=lhsT, rhs=rhs, start=start, stop=stop), reads=r, writes=w)

        def tr(out, in_, idn, r, w):
            S.add("pe", lambda e: e.transpose(out, in_, idn), reads=r, writes=w)

        def dma(q, out, in_, r, w, semkey, group=False):
            S.add(q, lambda e: e.dma_start(out=out, in_=in_), reads=r, writes=w, semkey=semkey, group=group)

        def memset(eng, ap, val, w):
            S.add(eng, lambda e: e.memset(ap, val), writes=w)

        regcache = {}

        def bnd(e):
            if "r" not in regcache:
                regcache["r"] = e.to_reg(NROWS - 1)
            return regcache["r"]

        def dump(name, ap, r):
            if debug:
                dma("pool", dbg[name], ap, r, ["dbg_" + name], "dbg_" + name)

        R1, R2, R3, R4 = 0, 64 * KB, 96 * KB, 140 * KB
        SB = af(R1 + 0, 512); SBp = af(R1 + 2 * KB, 512); SC = af(R1 + 4 * KB, 512); SCp = af(R1 + 6 * KB, 512)
        S0 = af(R1 + 8 * KB, 512); S0p = af(R1 + 10 * KB, 512); T1 = af(R1 + 12 * KB, 512); T2 = af(R1 + 14 * KB, 512)
        PBk = ab(R1 + 16 * KB, 8 * 512).rearrange("p (k c) -> p k c", c=512)
        qT = ab(R1 + 0, 4 * 2048).rearrange("p (g t) -> p g t", t=2048)
        kT = ab(R1 + 16 * KB, 2048)
        v1 = ab(R1 + 20 * KB, 16 * 2 * 66).rearrange("p (t k c) -> p t k c", k=2, c=66)
        uT = ab(R1 + 25 * KB, 4 * 2048).rearrange("p (j t) -> p j t", t=2048)
        gT = qT
        TB = R1 + 41 * KB
        xres = af(R1, NT * D).rearrange("p (t d) -> p t d", d=D)
        hT = ab(R2, 8 * 2048).rearrange("p (k t) -> p k t", t=2048)
        mixT = hT
        actT = hT
        Qd = ab(R3, 9 * 512).rearrange("p (k c) -> p k c", c=512)
        PTp = ab(R3 + 9 * KB, 4 * 4 * 8 * 128).rearrange("p (j a k c) -> p j a k c", a=4, k=8, c=128)
        MT = ab(R1 + 56 * KB, 4 * 8 * 128).rearrange("p (j k c) -> p j k c", k=8, c=128)
        s1L = af(R3 + 41 * KB, 256).rearrange("p (l g) -> p l g", g=32)
        s2L = af(R3 + 42 * KB, 256).rearrange("p (l g) -> p l g", g=32)
        slab = [ab(R3 + i * 4 * KB, 8 * 256).rearrange("p (k c) -> p k c", c=256) for i in range(3)]
        wdb = [ab(R3 + 12 * KB + i * 16 * KB, 8 * 1024).rearrange("p (k c) -> p k c", c=1024) for i in range(2)]

        dma("sp", cst[:], cst_d, [], ["cst"], "cstld", group=True)
        dma("sp", prm[:], prm_d, [], ["prm"], "cstld", group=True)
        dma("sp", SB, sb_d[:, 0, :], [], ["SB"], "cstld", group=True)
        dma("sp", SBp, sb_d[:, 1, :], [], ["SBp"], "cstld", group=True)
        dma("sp", SC, sc_d[:, 0, :], [], ["SC"], "cstld", group=True)
        dma("sp", SCp, sc_d[:, 1, :], [], ["SCp"], "cstld", group=True)
        win_b = ab(R4, 8 * 1280).rearrange("p (k c) -> p k c", c=1280)
        wada = af(TB, 8 * 256).rearrange("p (k c) -> p k c", c=256)
        xt = [af(R4 + 20 * KB + i * 4 * KB, 1024) for i in range(2)]
        xn = [af(R4 + 28 * KB + i * 4 * KB, 1024) for i in range(2)]
        qraw = [af(R4 + 36 * KB + i * 2 * KB, 512) for i in range(2)]
        sqb = [ab(R4 + 40 * KB + i * KB, 512) for i in range(2)]
        lnv = af(R4 + 42 * KB, 512)
        T_junk = ab(R4 + 44 * KB, 1024)
        lnv2 = [lnv, af(R4 + 46 * KB, 512)]
        nmask = ab(TB + 12 * KB, 2 * 512).rearrange("p (a c) -> p a c", c=512)
        dma("pool", win_b, win_d.rearrange("(k p) c -> p k c", p=128), [], ["win"], "winld")

        cp("dve", cbf[:, 0:384], cst[:, 0:384], ["cst"], ["cbf"])
        memset("dve", cbf[:, 512:640], 1.0, ["cbf1"])
        SM = lambda a, n: smalls[:, a:a + n]
        cact = SM(0, 8)
        modc = SM(8, 32).rearrange("p (s k) -> p s k", k=8)
        scale1 = SM(40, 8); scale2 = SM(48, 8)
        esink = SM(56, 8)
        bu1 = None
        ssq_a = SM(64, 16); std_a = SM(80, 16); rstd_a = SM(96, 16)
        ssq_b = SM(112, 16); std_b = SM(128, 16); rstd_b = SM(144, 16)
        ssq_d = SM(160, 16); std_d = SM(176, 16); rstd_d = SM(192, 16)
        den4 = SM(208, 8); rden4 = SM(216, 8)
        rt = SM(224, 64)
        cb2 = SM(288, 16)

        act(cact, prm[:, P_CCOL:P_CCOL + 8], AF.Silu, ["prm"], ["cact"])
        cact2 = SM(400, 16).rearrange("p (k c) -> p k c", c=2)
        cp("dve", cact2, cact.unsqueeze(2).broadcast_to([128, 8, 2]), ["cact"], ["cact"])
        act(esink, prm[:, P_SINK:P_SINK + 8], AF.Exp, ["prm"], ["esink"])

        def wada_load(sec, q):
            c0 = sec * 1024 + q * 256
            dma("sp", wada, wada_d[:, c0:c0 + 256].rearrange("(k p) c -> p k c", p=128), [], ["wada"], "wadald")

        cbT = af(TB + 8 * KB, 8 * 128).rearrange("p (k m) -> p k m", m=128)
        cp("dve", cbT, cact.unsqueeze(2).broadcast_to([128, 8, 128]), ["cact"], ["cbT"])

        def mod_cols(sec, slot, fin=True):
            for q in range(4):
                wada_load(sec, q)
                for fc in range(2):
                    col = slot * 8 + q * 2 + fc
                    for kc in range(8):
                        mm(bank(6)[:, 2 * col:2 * col + 2], wada[:, kc, fc * 128:(fc + 1) * 128], cact2[:, kc, :], kc == 0, kc == 7,
                           ["wada", "cact"], [PSK(6)])
            def _fin():
                tt("dve", modc[:, slot, :], bank(6)[:, slot * 16:slot * 16 + 16:2], prm[:, P_BADA + sec * 8:P_BADA + sec * 8 + 8], ALU.add,
                   [PSK(6), "prm"], ["modc%d" % slot])
            if fin:
                _fin()
            return _fin

        fin0 = mod_cols(0, 0, fin=False)
        fin1 = mod_cols(1, 1, fin=False)

        sw = af(R2, 32 * 40).rearrange("p (a g) -> p a g", g=32)
        SP1 = af(R2 + 5 * KB, 9 * 32).rearrange("p (k g) -> p k g", g=32)
        SP2 = af(R2 + 7 * KB, 9 * 32).rearrange("p (k g) -> p k g", g=32)
        SP1q = af(R2 + 9 * KB, 9 * 32).rearrange("p (k g) -> p k g", g=32)
        SP2q = af(R2 + 11 * KB, 9 * 32).rearrange("p (k g) -> p k g", g=32)
        lamr = prm[:, P_LAMR:P_LAMR + 32]
        lami = prm[:, P_LAMI:P_LAMI + 32]
        K_ = "ssmsetup"
        dt_ = sw[:, 0, :]; aa = sw[:, 1, :]; mag = sw[:, 2, :]; ang = sw[:, 3, :]
        act(dt_, prm[:, P_LDT:P_LDT + 32], AF.Exp, ["prm"], [K_])
        tt("dve", aa, lamr, dt_, ALU.mult, ["prm", K_], [K_])
        act(mag, aa, AF.Exp, [K_], [K_])
        tt("dve", ang, lami, dt_, ALU.mult, ["prm", K_], [K_])

        def sincos(dst, shift, base):
            t0 = sw[:, base, :]; t1 = sw[:, base + 1, :]; t2 = sw[:, base + 2, :]; t3 = sw[:, base + 3, :]
            ts("dve", t0, ang, shift, ALU.add, [K_], [K_])
            ts("dve", t1, t0, 1.0 / TWO_PI, ALU.mult, [K_], [K_])
            ts("dve", t2, t1, MAGIC, ALU.add, [K_], [K_])
            ts("dve", t2, t2, -MAGIC, ALU.add, [K_], [K_])
            stt(t3, t2, -TWO_PI, t0, ALU.mult, ALU.add, [K_], [K_])
            ts("dve", t3, t3, 3.14159, ALU.min, [K_], [K_], s2=-3.14159, op1=ALU.max)
            act(dst, t3, AF.Sin, [K_], [K_])

        sn = sw[:, 4, :]; cs = sw[:, 5, :]
        sincos(sn, 0.0, 8)
        sincos(cs, 0.5 * math.pi, 12)
        lbr = SP1[:, 1, :]; lbi_raw = sw[:, 6, :]
        tt("dve", lbr, mag, cs, ALU.mult, [K_], [K_])
        tt("dve", lbi_raw, mag, sn, ALU.mult, [K_], [K_])
        ts("dve", SP2[:, 1, :], lbi_raw, sgn, ALU.mult, [K_, "cst"], [K_])
        den = sw[:, 16, :]; t_a = sw[:, 17, :]; t_b = sw[:, 18, :]; rden = sw[:, 19, :]; lbm1 = sw[:, 20, :]
        cr = sw[:, 21, :]; ci = sw[:, 22, :]; c2 = sw[:, 23, :]
        tt("dve", t_a, lamr, lamr, ALU.mult, ["prm"], [K_])
        tt("dve", t_b, lami, lami, ALU.mult, ["prm", K_], [K_])
        tt("dve", den, t_a, t_b, ALU.add, [K_], [K_])
        S.add("dve", lambda e: e.reciprocal(out=rden, in_=den), reads=[K_], writes=[K_])
        ts("dve", lbm1, lbr, -1.0, ALU.add, [K_], [K_])
        tt("dve", t_a, lbm1, lamr, ALU.mult, [K_, "prm"], [K_])
        tt("dve", t_b, lbi_raw, lami, ALU.mult, [K_, "prm"], [K_])
        tt("dve", t_a, t_a, t_b, ALU.add, [K_], [K_])
        tt("dve", cr, t_a, rden, ALU.mult, [K_], [K_])
        tt("dve", t_a, lbi_raw, lamr, ALU.mult, [K_, "prm"], [K_])
        tt("dve", t_b, lbm1, lami, ALU.mult, [K_, "prm"], [K_])
        tt("dve", t_a, t_a, t_b, ALU.subtract, [K_], [K_])
        tt("dve", ci, t_a, rden, ALU.mult, [K_], [K_])
        ts("dve", c2, ci, sgn, ALU.mult, [K_, "cst"], [K_])

        def bc(v):
            return v.unsqueeze(2).broadcast_to([128, 32, 16])

        v3 = lambda a: a.rearrange("p (g h) -> p g h", h=16)
        tt("dve", v3(T1), v3(SB), bc(cr), ALU.mult, ["SB", K_], [K_])
        tt("dve", v3(T2), v3(SBp), bc(c2), ALU.mult, ["SBp", K_], [K_])
        tt("dve", S0, T1, T2, ALU.subtract, [K_], [K_])
        tt("dve", v3(T1), v3(SBp), bc(cr), ALU.mult, ["SBp", K_], [K_])
        tt("dve", v3(T2), v3(SB), bc(c2), ALU.mult, ["SB", K_], [K_])
        tt("dve", S0p, T1, T2, ALU.add, [K_], [K_])
        memset("dve", SP1[:, 0, :], 1.0, [K_])
        memset("dve", SP2[:, 0, :], 0.0, [K_])
        for k in range(1, 8):
            tt("dve", t_a, SP1[:, k, :], SP1[:, 1, :], ALU.mult, [K_], [K_])
            tt("dve", t_b, SP2[:, k, :], SP2[:, 1, :], ALU.mult, [K_], [K_])
            tt("dve", SP1[:, k + 1, :], t_a, t_b, ALU.subtract, [K_], [K_])
            tt("dve", t_a, SP1[:, k, :], SP2[:, 1, :], ALU.mult, [K_], [K_])
            tt("dve", t_b, SP2[:, k, :], SP1[:, 1, :], ALU.mult, [K_], [K_])
            tt("dve", SP2[:, k + 1, :], t_a, t_b, ALU.add, [K_], [K_])
        ts("dve", SP1q.rearrange("p k g -> p (k g)"), SP1.rearrange("p k g -> p (k g)"), sgn, ALU.mult, [K_, "cst"], [K_])
        ts("dve", SP2q.rearrange("p k g -> p (k g)"), SP2.rearrange("p k g -> p (k g)"), sgn, ALU.mult, [K_, "cst"], [K_])
        cp("dve", s1L[:, 0, :], SP1[:, 8, :], [K_], ["sL"])
        cp("dve", s2L[:, 0, :], SP2[:, 8, :], [K_], ["sL"])
        for l in range(7):
            tt("dve", t_a, s1L[:, l, :], s1L[:, l, :], ALU.mult, ["sL", K_], [K_])
            tt("dve", t_b, s2L[:, l, :], s2L[:, l, :], ALU.mult, ["sL", K_], [K_])
            tt("dve", s1L[:, l + 1, :], t_a, t_b, ALU.subtract, [K_], ["sL"])
            tt("dve", t_a, s1L[:, l, :], s2L[:, l, :], ALU.mult, ["sL", K_], [K_])
            ts("dve", s2L[:, l + 1, :], t_a, 2.0, ALU.mult, [K_], ["sL"])
        dump("d_sp", SP1.rearrange("p k g -> p (k g)"), [K_])
        for k in range(9):
            if k < 8:
                tt("dve", v3(T1), v3(S0), bc(SP1[:, k, :]), ALU.mult, [K_], [K_])
                tt("dve", v3(T2), v3(S0p), bc(SP2[:, k, :]), ALU.mult, [K_], [K_])
                tt("dve", T1, T1, T2, ALU.subtract, [K_], [K_])
                cp("act", PBk[:, k, :], T1, [K_], ["PBk"])
            tt("dve", v3(T1), v3(SC), bc(SP1q[:, k, :]), ALU.mult, ["SC", K_], [K_])
            tt("dve", v3(T2), v3(SCp), bc(SP2q[:, k, :]), ALU.mult, ["SCp", K_], [K_])
            tt("dve", T1, T1, T2, ALU.subtract, [K_], [K_])
            cp("act", Qd[:, k, :], T1, [K_], ["Qd"])
        for j in range(4):
            for k in range(8):
                bi = (j * 8 + k) % 2
                tr(bankb(bi)[:, 0:128], PBk[:, k, j * 128:(j + 1) * 128], ident_b, ["PBk", "cbf"], [PSK(bi)])
                for a_ in range(4):
                    ts("dve", PTp[:, j, a_, k, :], bankb(bi)[:, 0:128], cst[:, C_ROWM4 + a_:C_ROWM4 + a_ + 1], ALU.mult,
                       [PSK(bi), "cst"], ["PTp"])
        for j in range(4):
            jsl = slice(j * 128, (j + 1) * 128)
            for dl in range(8):
                bi = 2 + (j * 8 + dl) % 2
                mm(bank(bi)[:, 0:128], PBk[:, dl, jsl], Qd[:, 0, jsl], True, True, ["PBk", "Qd"], [PSK(bi)])
                tt("dve", MT[:, j, dl, :], bank(bi)[:, 0:128], cst[:, C_BD16:C_BD16 + 128], ALU.mult, [PSK(bi), "cst"], ["MT"])
                if dl == 0:
                    stt(MT[:, j, 0, :], identf, prm[:, P_DSK + j:P_DSK + j + 1], MT[:, j, 0, :], ALU.mult, ALU.add, ["MT", "cst", "prm"], ["MT"])

        fin0()
        fin1()
        stt(scale1, modc[:, 1, :], 1.0, prm[:, P_NMIX:P_NMIX + 8], ALU.add, ALU.mult, ["modc1", "prm"], ["scale1"])
        S.mark('setup')
        S.barrier()
        S.mark('ada')
        memset("pool", v1[:, :, :, 64:65], 1.0, ["v1ones"])

        def norm_tile(t, xsrc, rk, ssq, std, rstd, scale, bias, dstT, dst_dt_key, xnbuf, pbanks, extra=None):
            act(T_junk, xsrc, AF.Square, rk, ["junk"], accum=ssq[:, t:t + 1])
            act(std[:, t:t + 1], ssq[:, t:t + 1], AF.Sqrt, ["junk"], ["std"], scale=1.0 / D, bias=epsc)
            S.add("dve", lambda e: e.reciprocal(out=rstd[:, t:t + 1], in_=std[:, t:t + 1]), reads=["std"], writes=["rstd"])
            ts("dve", xnbuf, xsrc, rstd[:, t:t + 1], ALU.mult, rk + ["rstd"], [("xn", id(xnbuf))])
            for kc in range(8):
                b_ = pbanks[kc // 4]
                tr(bank(b_)[:, (kc % 4) * 128:(kc % 4) * 128 + 128], xnbuf[:, kc * 128:(kc + 1) * 128], identf,
                   [("xn", id(xnbuf)), "cst"], [PSK(b_)])

        bias1 = modc[:, 0, :]
        NCH = 9
        for t in range(NT):
            b = t % 2
            dma("sp", xt[b], x_d[t * 128:(t + 1) * 128, :], [], [("xt", b)], "xtld%d" % b)
            pb = (0, 1) if b == 0 else (2, 3)
            norm_tile(t, xt[b], [("xt", b)], ssq_a, std_a, rstd_a, scale1, bias1, hT, None, xn[b], pb)
            tb = t // 4
            for kc in range(8):
                b_ = pb[kc // 4]
                act(hT[:, kc, t * 128:(t + 1) * 128], bank(b_)[:, (kc % 4) * 128:(kc % 4) * 128 + 128], AF.Identity,
                    [PSK(b_), "scale1", "modc0"], [("hT", tb)], scale=scale1[:, kc:kc + 1], bias=bias1[:, kc:kc + 1])
            for kc in range(8):
                mm(bank(7)[:, 0:128], hT[:, kc, t * 128:(t + 1) * 128], win_b[:, kc, 1152:1280], kc == 0, kc == 7,
                   [("hT", tb), "win"], [PSK(7)])
            tt("dve", v1[:, t, :, 0:64], bank(7)[:, 0:128].rearrange("p (k c) -> p k c", c=64),
               prm[:, P_BVROW:P_BVROW + 128].rearrange("p (k c) -> p k c", c=64), ALU.add, [PSK(7), "prm"], [("v1", t)])
            if t % 4 == 3:
                tsl = slice(tb * 512, tb * 512 + 512)
                def s0(oc):
                    pbk = 4 + oc % 2
                    for kc in range(8):
                        mm(bank(pbk), win_b[:, kc, oc * 128:(oc + 1) * 128], hT[:, kc, tsl], kc == 0, kc == 7, [("hT", tb), "win"], [PSK(pbk)])

                def s1(oc):
                    pbk = 4 + oc % 2
                    bcol = prm[:, P_BIN + oc:P_BIN + oc + 1]
                    if oc < 5:
                        qi = oc % 2
                        act(qraw[qi], bank(pbk), AF.Identity, [PSK(pbk), "prm"], [("qraw", qi)], bias=bcol)
                        act(sqb[qi], bank(pbk), AF.Square, [PSK(pbk), "prm"], [("sqb", qi)], bias=bcol)
                        mm(bank(6 + qi), bones_b, sqb[qi], True, True, [("sqb", qi), "cbf"], [PSK(6 + qi)])
                    else:
                        act(uT[:, oc - 5, tsl], bank(pbk), AF.Identity, [PSK(pbk), "prm"], [("uT", tb)], bias=bcol)

                def s2(oc):
                    if oc >= 5:
                        return
                    qi = oc % 2
                    lv = lnv2[qi]
                    act(lv, bank(6 + qi), AF.Ln, [PSK(6 + qi)], [("lnv", qi)], scale=1.0 / 64, bias=epsc)
                    act(lv, lv, AF.Exp, [("lnv", qi)], [("lnv", qi)], scale=-0.5)
                    dst = qT[:, oc, tsl] if oc < 4 else kT[:, tsl]
                    gcol = prm[:, P_QG:P_QG + 1] if oc < 4 else prm[:, P_QG + 1:P_QG + 2]
                    stt(dst, qraw[qi], gcol, lv, ALU.mult, ALU.mult, [("qraw", qi), ("lnv", qi), "prm"], [("qk", oc, tb)])

                for step in range(NCH + 2):
                    if step < NCH:
                        s0(step)
                    if 0 <= step - 1 < NCH:
                        s1(step - 1)
                    if 0 <= step - 2 < NCH:
                        s2(step - 2)
        dump("d_hT", hT.rearrange("p k t -> p (k t)"), [("hT", i) for i in range(4)])
        dump("d_qT", qT.rearrange("p g t -> p (g t)"), [("qk", oc, tb) for oc in range(4) for tb in range(4)])
        dump("d_kT", kT, [("qk", 4, tb) for tb in range(4)])
        dump("d_uT", uT.rearrange("p j t -> p (j t)"), [("uT", i) for i in range(4)])
        dump("d_v1", v1.rearrange("p t k c -> p (t k c)"), [("v1", t) for t in range(NT)] + ["v1ones"])

        grow = st.enter_context(nc.sbuf_tensor("grow", [128, 2, 1024], BF16))
        wadaL = af(R4, 8 * 256).rearrange("p (k c) -> p k c", c=256)
        cbTL = af(R4 + 8 * KB, 8 * 128).rearrange("p (k m) -> p k m", m=128)
        bgrL = af(R4 + 12 * KB, 256)
        late_units = []

        def _wl(sec, q):
            c0 = sec * 1024 + q * 256
            dma("sp", wadaL, wada_d[:, c0:c0 + 256].rearrange("(k p) c -> p k c", p=128), [], ["wadaL"], "wadaLld")

        def _mk_col_unit(sec, slot, q):
            def u():
                if sec == 3 and q == 0:
                    cp("dve", cbTL, cact.unsqueeze(2).broadcast_to([128, 8, 128]), ["cact"], ["cbTL"])
                _wl(sec, q)
                for fc in range(2):
                    col = slot * 8 + q * 2 + fc
                    for kc in range(8):
                        mm(bank(7)[:, 256 + 2 * col:256 + 2 * col + 2], wadaL[:, kc, fc * 128:(fc + 1) * 128], cact2[:, kc, :], kc == 0, kc == 7,
                           ["wadaL", "cact"], [PSK(7)])
                if q == 3:
                    tt("dve", modc[:, slot, :], bank(7)[:, 256 + slot * 16:256 + slot * 16 + 16:2], prm[:, P_BADA + sec * 8:P_BADA + sec * 8 + 8],
                       ALU.add, [PSK(7), "prm"], ["modc%d" % slot, PSK(7)])
                    if slot == 3:
                        stt(scale2, modc[:, 3, :], 1.0, prm[:, P_NFFN:P_NFFN + 8], ALU.add, ALU.mult, ["modc3", "prm"], ["scale2"])
            return u

        def _mk_row_unit(gi_, sec, q):
            def u():
                _wl(sec, q)
                dma("sp", bgrL, badag_d[:, gi_ * 1024 + q * 256: gi_ * 1024 + q * 256 + 256], [], ["bgrL"], "bgrLld")
                for kc in range(8):
                    mm(bank(6)[:, 256:512], cbTL[:, kc, :], wadaL[:, kc, :], kc == 0, kc == 7, ["wadaL", "cbTL"], [PSK(6)])
                tt("dve", grow[:, gi_, q * 256:(q + 1) * 256], bank(6)[:, 256:512], bgrL, ALU.add, [PSK(6), "bgrL"], [("grow", gi_), PSK(6)])
            return u
        for sec, slot in ((3, 2), (4, 3)):
            for q in range(4):
                late_units.append(_mk_col_unit(sec, slot, q))
        for gi_, sec in enumerate((2, 5)):
            for q in range(4):
                late_units.append(_mk_row_unit(gi_, sec, q))

        S.mark('A')
        S.barrier()
        for a_ in range(2):
            cp("dve", nmask[:, a_, :].rearrange("p (h t) -> p h t", t=128),
               cst[:, C_NMCUR + 128 * a_: C_NMCUR + 128 * a_ + 128].unsqueeze(1).broadcast_to([128, 4, 128]), ["cst"], ["nmask"])

        Eb = [ab(TB + i * KB, 512) for i in range(4)]
        attn_f = [af(TB + 4 * KB + i * 2 * KB, 512) for i in range(2)]
        attn_b = [ab(TB + 8 * KB + i * KB, 512) for i in range(2)]
        T_junk2 = ab(TB + 10 * KB, 512)
        ecount = 0
        for n in range(NT):
            ab_ = n % 2
            for kv in range(2):
                prt = slice(kv * 64, kv * 64 + 64)
                blocks = [(n, 0)] + ([(n - 1, 1)] if n > 0 else [])
                ebufs = []
                for (kb, mi) in blocks:
                    pbk = ecount % 4
                    eb = Eb[ecount % 4]
                    ecount += 1
                    mm(bank(pbk), kT[prt, kb * 128:(kb + 1) * 128], qT[prt, :, n * 128:(n + 1) * 128], True, False,
                       [("qk", 4, kb // 4)] + [("qk", oc, n // 4) for oc in range(4)], [PSK(pbk)])
                    mm(bank(pbk), ident_b, nmask[:, mi, :], False, True, ["cbf", "nmask"], [PSK(pbk)])
                    act(eb, bank(pbk), AF.Exp, [PSK(pbk)], [("E", id(eb))], scale=0.125)
                    ebufs.append((eb, kb))
                pob = 4 + (n * 2 + kv) % 2
                po = bank(pob)[:, 0:4 * 66].rearrange("p (h c) -> p h c", c=66)
                for h in range(4):
                    for i_, (eb, kb) in enumerate(ebufs):
                        mm(po[:, h, 0:65], eb[:, h * 128:(h + 1) * 128], v1[:, kb, kv, 0:65], i_ == 0, i_ == len(ebufs) - 1,
                           [("E", id(eb)), ("v1", kb), "v1ones"], [PSK(pob)])
                dk = den4[:, kv * 4:kv * 4 + 4]
                rk_ = rden4[:, kv * 4:kv * 4 + 4]
                tt("dve", dk, po[:, :, 64], esink[:, kv * 4:kv * 4 + 4], ALU.add, [PSK(pob), "esink"], [("den", kv)])
                S.add("dve", lambda e, dk=dk, rk_=rk_: e.reciprocal(out=rk_, in_=dk), reads=[("den", kv)], writes=[("rden", kv)])
                tt("dve", attn_f[ab_][:, kv * 256:(kv + 1) * 256].rearrange("p (h c) -> p h c", c=64), po[:, :, 0:64],
                   rk_.unsqueeze(2).broadcast_to([128, 4, 64]), ALU.mult, [PSK(pob), ("rden", kv)], [("attnf", ab_)])
            act(T_junk2, attn_f[ab_], AF.Square, [("attnf", ab_)], ["junk2"], accum=ssq_b[:, n:n + 1])
            act(std_b[:, n:n + 1], ssq_b[:, n:n + 1], AF.Sqrt, ["junk2"], ["std_b"], scale=1.0 / 512, bias=epsc)
            S.add("dve", lambda e, n=n: e.reciprocal(out=rstd_b[:, n:n + 1], in_=std_b[:, n:n + 1]), reads=["std_b"], writes=["rstd_b"])
            ts("dve", attn_b[ab_], attn_f[ab_], rstd_b[:, n:n + 1], ALU.mult, [("attnf", ab_), "rstd_b"], [("attnb", ab_)])
            tbk = 6 + n % 2
            for c4 in range(4):
                tr(bankb(tbk)[:, c4 * 128:(c4 + 1) * 128], attn_b[ab_][:, c4 * 128:(c4 + 1) * 128], ident_b, [("attnb", ab_), "cbf"], [PSK(tbk)])
            cp("act", mixT[:, 0:4, n * 128:(n + 1) * 128], bankb(tbk)[:, 0:512].rearrange("p (c t) -> p c t", t=128), [PSK(tbk)], [("mixA", n)])
            late_units[n]()
        dump("d_mod", smalls[:, 0:64], ["modc0", "modc1", "modc2", "modc3", "scale1", "scale2", "cact"])
        dump("d_g1", grow[:].rearrange("p a c -> p (a c)"), [("grow", 0), ("grow", 1)])

        S.mark('B')
        S.barrier()
        X32 = af(R4, 32 * 257).rearrange("p (g c) -> p g c", c=257)
        Xbf = ab(R4 + 33 * KB, 32 * 258).rearrange("p (g c) -> p g c", c=258)
        Abuf = [ab(TB + i * 2 * KB, 8 * 128).rearrange("p (g c) -> p g c", c=128) for i in range(4)]
        At1g = [af(TB + 8 * KB + i * 512, 128) for i in range(4)]
        wglu_b = ab(TB + 10 * KB, 4 * 512).rearrange("p (k c) -> p k c", c=512)
        sigb = [ab(R1 + 19 * KB + i * KB, 512) for i in range(2)]
        sq4 = ab(R1 + 21 * KB, 4 * 512).rearrange("p (k c) -> p k c", c=512)
        rstd_c = af(R1 + 16 * KB, 512)
        Ytok = [ab(R1 + 18 * KB + i * 256, 128) for i in range(2)]
        dma("pool", wglu_b, wglu_d.rearrange("(k p) c -> p k c", p=128), [], ["wglu"], "wgluld")
        memset("dve", X32[:, :, 0:1], 0.0, ["X32z"])
        memset("dve", Xbf[:, :, 0:1], 0.0, ["Xbfz"])
        for g in range(32):
            j, gi = g // 8, g % 8
            hf, a_ = gi // 4, gi % 4
            pbk = g % 4
            prt = slice(64 * hf, 64 * hf + 64)
            for tau in range(8):
                mm(bank(pbk)[:, 0:256], PTp[prt, j, a_, 7 - tau, :], uT[prt, j, tau:2048:8], tau == 0, tau == 7,
                   ["PTp"] + [("uT", i) for i in range(4)], [PSK(pbk)])
            cp("act", X32[:, g, 1:257], bank(pbk)[:, 0:256], [PSK(pbk)], [("X32", j)])
            cp("dve", Xbf[:, g, 1:257], X32[:, g, 1:257], [("X32", j)], [("Xbf", j)])
        ps_sets = [psum[:, 0:2048].rearrange("p (g c) -> p g c", c=256), psum[:, 2048:4096].rearrange("p (g c) -> p g c", c=256)]
        for l in range(8):
            d_ = 1 << l
            for j in range(4):
                A_ = Abuf[j]
                for gi in range(8):
                    g = 8 * j + gi
                    a1 = At1g[gi % 4]
                    act(a1, identf, AF.Identity, ["cst", "sL"], [("At1g", gi % 4)], scale=s1L[:, l, g:g + 1])
                    stt(A_[:, gi, :], shiftf, s2L[:, l, g:g + 1], a1, ALU.mult, ALU.add, ["cst", "sL", ("At1g", gi % 4)], [("A", j)])
            for j in range(4):
                pset = ps_sets[j % 2]
                b0 = 4 * (j % 2)
                for gi in range(8):
                    g = 8 * j + gi
                    mm(pset[:, gi, 0:256 - d_], Abuf[j][:, gi, :], Xbf[:, g, 1:257 - d_], True, True, [("A", j), ("Xbf", j)],
                       [PSK(b0 + gi // 2)])
                tt("dve", X32[:, 8 * j:8 * j + 8, 1 + d_:257], X32[:, 8 * j:8 * j + 8, 1 + d_:257], pset[:, :, 0:256 - d_], ALU.add,
                   [PSK(b0), PSK(b0 + 1), PSK(b0 + 2), PSK(b0 + 3), ("X32", j)], [("X32", j)])
                cp("act", Xbf[:, 8 * j:8 * j + 8, 1 + d_:257], X32[:, 8 * j:8 * j + 8, 1 + d_:257], [("X32", j)], [("Xbf", j)])
        dump("d_x32", X32.rearrange("p g c -> p (g c)"), [("X32", j) for j in range(4)])
        yc = 0
        for j in range(4):
            for tau in range(8):
                pbk = yc % 4
                yc += 1
                for sg_ in range(tau + 1):
                    mm(bank(pbk)[:, 0:256], MT[:, j, tau - sg_, :], uT[:, j, sg_:2048:8], sg_ == 0, False,
                       ["MT"] + [("uT", i) for i in range(4)], [PSK(pbk)])
                for hf in range(2):
                    pt_ = 4 + (yc * 2 + hf) % 4
                    yt = Ytok[(yc * 2 + hf) % 2]
                    for gi in range(8):
                        g = 8 * j + gi
                        mm(bank(pt_)[:, gi * 16:(gi + 1) * 16], Xbf[:, g, hf * 128:(hf + 1) * 128], Qd[:, tau + 1, g * 16:(g + 1) * 16], True, True,
                           ["Qd", ("Xbf", j), "Xbfz"], [PSK(pt_)])
                    cp("dve", yt, bank(pt_)[:, 0:128], [PSK(pt_)], [("Ytok", id(yt))])
                    mm(bank(pbk)[:, hf * 128:(hf + 1) * 128], yt, ident_b, False, hf == 1, [("Ytok", id(yt)), "cbf"], [PSK(pbk)])
                act(gT[:, j, tau:2048:8], bank(pbk)[:, 0:256], AF.Gelu_apprx_tanh, [PSK(pbk)], [("gT", j)])
        dump("d_gT", gT.rearrange("p j t -> p (j t)"), [("gT", j) for j in range(4)])
        gc_ = 0
        for tb in range(4):
            tsl = slice(tb * 512, tb * 512 + 512)
            for oc in range(4):
                pbk = 4 + gc_ % 2
                sb_ = sigb[gc_ % 2]
                gc_ += 1
                for kc in range(4):
                    mm(bank(pbk), wglu_b[:, kc, oc * 128:(oc + 1) * 128], gT[:, kc, tsl], kc == 0, kc == 3,
                       ["wglu"] + [("gT", j) for j in range(4)], [PSK(pbk)])
                act(sb_, bank(pbk), AF.Sigmoid, [PSK(pbk), "prm"], [("sig", id(sb_))], bias=prm[:, P_BGLU + oc:P_BGLU + oc + 1])
                tt("dve", mixT[:, 4 + oc, tsl], gT[:, oc, tsl], sb_, ALU.mult, [("sig", id(sb_)), ("gT", oc)], [("mixS", tb)])
            act(sq4, mixT[:, 4:8, tsl], AF.Square, [("mixS", tb)], ["sq4"])
            for oc in range(4):
                mm(bank(6), ones_b, sq4[:, oc, :], oc == 0, oc == 3, ["sq4", "cbf1"], [PSK(6)])
            act(rstd_c, bank(6), AF.Ln, [PSK(6)], ["rstd_c"], scale=1.0 / 512, bias=epsc)
            act(rstd_c, rstd_c, AF.Exp, ["rstd_c"], ["rstd_c"], scale=-0.5)
            tt("dve", mixT[:, 4:8, tsl], mixT[:, 4:8, tsl], rstd_c.unsqueeze(1).broadcast_to([128, 4, 512]), ALU.mult,
               [("mixS", tb), "rstd_c"], [("mixS", tb)])
        dump("d_mix", mixT.rearrange("p k t -> p (k t)"), [("mixA", n) for n in range(NT)] + [("mixS", tb) for tb in range(4)])

        S.mark('C')
        S.barrier()
        wout_b = ab(R3 + 8 * KB, 8 * 1024).rearrange("p (k c) -> p k c", c=1024)
        rowS = af(R4, 1024)
        rowB = af(R4 + 4 * KB, 1024)
        h2tmp = af(R4 + 8 * KB, 1024)
        h2tok = [ab(R4 + 12 * KB + i * 2 * KB, 1024) for i in range(2)]
        Mbf = ab(R4 + 16 * KB, 16 * 32).rearrange("p (t e) -> p t e", e=32)
        onesf = af(R4 + 17 * KB, 128)
        utf = af(R4 + 17 * KB + 512, 128)
        utb = ab(R4 + 18 * KB, 128)
        iota32 = af(R4 + 18 * KB + 256, 32)
        diag = [af(R4 + 19 * KB + i * 512, 128) for i in range(2)]
        misc = af(R4 + 20 * KB, 256)
        misc2 = af(R4 + 21 * KB, 256)
        posf_all = af(R4 + 22 * KB, 512).rearrange("p (t e) -> p t e", e=32)
        ef_all = af(R4 + 24 * KB, 64).rearrange("p (t k) -> p t k", k=4)
        rk = af(R4 + 25 * KB, 256)
        hbRow = af(R4 + 26 * KB, 32)
        hcRow = af(R4 + 26 * KB + 128, 32)
        xt2s = [af(R4 + 32 * KB, 1024), af(R4 + 44 * KB, 1024)]
        xn2s = [af(R4 + 36 * KB, 1024), af(R4 + 27 * KB, 1024)]
        h2fs = [af(R4 + 40 * KB, 1024).rearrange("p (k t) -> p k t", t=128), af(R3 + 24 * KB, 1024).rearrange("p (k t) -> p k t", t=128)]
        bdp = af(R3, 1024)
        GTt = af(R3 + 4 * KB, 128)
        wr_f = af(R3 + 5 * KB, 8 * 32).rearrange("p (k c) -> p k c", c=32)
        T_junk3 = ab(R3 + 6 * KB, 1024)
        dma("pool", wout_b, wout_d.rearrange("(k p) c -> p k c", p=128), [], ["wout"], "woutld")
        dma("sp", wr_f, wr_d.rearrange("(k p) c -> p k c", p=128), [], ["wr"], "wrld")
        dma("sp", bdp[0:32, :], bd_d, [], ["bdp"], "bdld")
        for kc in range(8):
            stt(wout_b[:, kc, :], wout_b[:, kc, :], prm[:, P_GCAT + kc:P_GCAT + kc + 1], grow[:, 0, :], ALU.mult, ALU.mult,
                ["wout", "prm", ("grow", 0)], ["wout"])
        tt("dve", bdp[0:32, :], bdp[0:32, :], grow[0:32, 1, :], ALU.mult, ["bdp", ("grow", 1)], ["bdp"])
        bias2 = modc[:, 2, :]
        memset("pool", onesf, 1.0, ["onesf"])
        memset("pool", utf, 1.0, ["utf"])
        S.add("pool", lambda e: e.affine_select(out=utf, in_=utf, pattern=[[1, 128]], compare_op=ALU.is_gt, fill=0.0, base=0,
                                               channel_multiplier=-1), reads=["utf"], writes=["utf"])
        cp("dve", utb, utf, ["utf"], ["utb"])
        S.add("pool", lambda e: e.iota(iota32, pattern=[[1, 32]], base=0, channel_multiplier=0, allow_small_or_imprecise_dtypes=True),
              writes=["iota32"])
        for which, (colv, rowv, ck) in enumerate(((scale2, rowS, "scale2"), (bias2, rowB, "modc2"))):
            for kc in range(8):
                dg = diag[kc % 2]
                ts("dve", dg, identf, colv[:, kc:kc + 1], ALU.mult, ["cst", ck], [("diag", kc % 2)])
                bk = 2 + kc // 4
                mm(bank(bk)[:, (kc % 4) * 128:(kc % 4) * 128 + 128], onesf, dg, True, True, ["onesf", ("diag", kc % 2)], [PSK(bk)])
            for h_ in range(2):
                cp("act", rowv[:, h_ * 512:(h_ + 1) * 512], bank(2 + h_), [PSK(2 + h_)], ["row%d" % which])
        def stageA(t):
                xt2 = xt2s[t % 2]; xn2 = xn2s[t % 2]; h2f = h2fs[t % 2]
                XT = ("xt2", t % 2); XN = ("xn2", t % 2); HF = ("h2f", t % 2)
                dma("sp", xt2, x_d[t * 128:(t + 1) * 128, :], [], [XT], "xt2ld%d" % (t % 2))
                for nb in range(2):
                    for kc in range(8):
                        mm(bank(nb), mixT[:, kc, t * 128:(t + 1) * 128], wout_b[:, kc, nb * 512:(nb + 1) * 512], kc == 0, kc == 7,
                           ["wout"], [PSK(nb)])
                    tt("dve", xres[:, t, nb * 512:(nb + 1) * 512], bank(nb), xt2[:, nb * 512:(nb + 1) * 512], ALU.add, [PSK(nb), XT], [("xres", t)])
                act(T_junk3, xres[:, t, :], AF.Square, [("xres", t)], ["junk3"], accum=ssq_d[:, t:t + 1])
                act(std_d[:, t:t + 1], ssq_d[:, t:t + 1], AF.Sqrt, ["junk3"], ["std_d"], scale=1.0 / D, bias=epsc)
                S.add("dve", lambda e, t=t: e.reciprocal(out=rstd_d[:, t:t + 1], in_=std_d[:, t:t + 1]), reads=["std_d"], writes=["rstd_d"])
                ts("dve", xn2, xres[:, t, :], rstd_d[:, t:t + 1], ALU.mult, [("xres", t), "rstd_d"], [XN])

        def stageB(t):
                xt2 = xt2s[t % 2]; xn2 = xn2s[t % 2]; h2f = h2fs[t % 2]
                XT = ("xt2", t % 2); XN = ("xn2", t % 2); HF = ("h2f", t % 2)
                for kc in range(8):
                    b_ = 2 + kc // 4
                    tr(bank(b_)[:, (kc % 4) * 128:(kc % 4) * 128 + 128], xn2[:, kc * 128:(kc + 1) * 128], identf, [XN, "cst"], [PSK(b_)])
                for kc in range(8):
                    b_ = 2 + kc // 4
                    act(h2f[:, kc, :], bank(b_)[:, (kc % 4) * 128:(kc % 4) * 128 + 128], AF.Identity, [PSK(b_), "scale2", "modc2"], [HF],
                        scale=scale2[:, kc:kc + 1], bias=bias2[:, kc:kc + 1])
                for kc in range(8):
                    mm(bank(4)[:, 0:32], h2f[:, kc, :], wr_f[:, kc, :], kc == 0, kc == 7, [HF, "wr"], [PSK(4)])
                lg = rt[:, 0:32]; top8 = rt[:, 32:40]; msk = rt[:, 40:72] if False else smalls[:, 304:336]
                ex = smalls[:, 336:368]; nmx = smalls[:, 368:369]; ssum = smalls[:, 369:370]; rsum = smalls[:, 370:371]
                tt("dve", lg, bank(4)[:, 0:32], prm[:, P_BROUT:P_BROUT + 32], ALU.add, [PSK(4), "prm"], ["lg"])
                S.add("dve", lambda e, lg=lg, top8=top8: e.max(out=top8, in_=lg), reads=["lg"], writes=["top8"])
                ts("dve", msk, lg, top8[:, 3:4], ALU.is_ge, ["lg", "top8"], ["msk"])
                ts("dve", nmx, top8[:, 0:1], -1.0, ALU.mult, ["top8"], ["nmx"])
                act(ex, lg, AF.Exp, ["lg", "nmx"], ["ex"], bias=nmx)
                tt("dve", ex, ex, msk, ALU.mult, ["ex", "msk"], ["ex"])
                S.add("dve", lambda e, ex=ex, ssum=ssum: e.tensor_reduce(out=ssum, in_=ex, axis=AX.X, op=ALU.add), reads=["ex"], writes=["ssum"])
                S.add("dve", lambda e, ssum=ssum, rsum=rsum: e.reciprocal(out=rsum, in_=ssum), reads=["ssum"], writes=["rsum"])
                ts("dve", Gt[:, t, :], ex, rsum, ALU.mult, ["ex", "rsum"], [("G", t)])
                S.add("dve", lambda e, lg=lg, top8=top8: e.max_index(out=idx8[:], in_max=top8, in_values=lg), reads=["lg", "top8"], writes=["idx8"])
                ex4 = misc[:, 88:92]
                cp("dve", ef_all[:, t, :], idx8[:, 0:4], ["idx8"], [("ef", t)])
                cp("dve", Mbf[:, t, :], msk, ["msk"], [("Mbf", t)])
                for i2 in range(t + 1):
                    mm(bank(4)[:, 64:96], ones_b if i2 < t else utb, Mbf[:, i2, :], i2 == 0, i2 == t,
                       [("Mbf", i2), "cbf1", "utb"], [PSK(4)])
                cp("dve", posf_all[:, t, :], bank(4)[:, 64:96], [PSK(4)], [("posf", t)])
                act(ex4, top8[:, 0:4], AF.Exp, ["top8", "nmx"], ["ex4"], bias=nmx)
                ts("dve", Gk[:, t, :], ex4, rsum, ALU.mult, ["ex4", "rsum"], [("Gk", t)])

        stageA(0)
        for t in range(NT):
            if t + 1 < NT:
                stageA(t + 1)
            stageB(t)
        allM = [("Mbf", i) for i in range(NT)]
        for i in range(NT):
            mm(bank(5)[0:32, 0:2], Mbf[:, i, :], ones_b[:, 0:2], i == 0, i == NT - 1, allM + ["cbf1"], [PSK(5)])
        for i in range(NT):
            mm(bank(5)[:, 64:96], ones_b, Mbf[:, i, :], i == 0, i == NT - 1, allM + ["cbf1"], [PSK(5)])
        cntc = rk[0:32, 0:1]; cntr = rk[0:32, 32:64]; gt_ = rk[0:32, 64:96]; eq_ = rk[0:32, 96:128]; low_ = rk[0:32, 128:160]
        rankc = rk[0:32, 160:161]; Rm = rk[0:32, 168:168 + HOTN]; tmp8 = rk[0:32, 176:176 + HOTN]; hbc = rk[0:32, 184:185]; hcc = rk[0:32, 185:186]
        ish = rk[0:32, 186:187]; dg32 = rk[0:32, 192:224]
        pidx = cst[0:32, C_PIDX:C_PIDX + 1]
        cp("dve", cntc, bank(5)[0:32, 0:1], [PSK(5)], ["rk"])
        cp("dve", cntr, bank(5)[0:32, 64:96], [PSK(5), "rk"], ["rk"])
        ts("dve", gt_, cntr, cntc, ALU.is_gt, ["rk"], ["rk"])
        ts("dve", eq_, cntr, cntc, ALU.is_equal, ["rk"], ["rk"])
        ts("dve", low_, iota32[0:32, :], pidx, ALU.is_lt, ["iota32", "cst", "rk"], ["rk"])
        tt("dve", eq_, eq_, low_, ALU.mult, ["rk"], ["rk"])
        tt("dve", gt_, gt_, eq_, ALU.add, ["rk"], ["rk"])
        S.add("dve", lambda e: e.tensor_reduce(out=rankc, in_=gt_, axis=AX.X, op=ALU.add), reads=["rk"], writes=["rk"])
        ts("dve", Rm, iota32[0:32, 0:HOTN], rankc, ALU.is_equal, ["rk", "iota32"], ["rk"])
        mm(bank(5)[0:1, 128:128 + HOTN], pidx, Rm, True, True, ["rk", "cst"], [PSK(5)])
        cp("dve", pi_i[0:1, 0:HOTN], bank(5)[0:1, 128:128 + HOTN], [PSK(5)], ["pi_i"])
        tt("dve", tmp8, Rm, cst[0:32, C_HB:C_HB + HOTN], ALU.mult, ["rk", "cst"], ["rk"])
        S.add("dve", lambda e: e.tensor_reduce(out=hbc, in_=tmp8, axis=AX.X, op=ALU.add), reads=["rk"], writes=["rk"])
        S.add("dve", lambda e: e.tensor_reduce(out=ish, in_=Rm, axis=AX.X, op=ALU.add), reads=["rk"], writes=["rk"])
        ts("dve", ish, ish, -1.0e6, ALU.mult, ["rk"], ["rk"], s2=1.0e6, op1=ALU.add)
        tt("dve", hbc, hbc, ish, ALU.add, ["rk"], ["rk"])
        tt("dve", tmp8, Rm, cst[0:32, C_HC:C_HC + HOTN], ALU.mult, ["rk", "cst"], ["rk"])
        S.add("dve", lambda e: e.tensor_reduce(out=hcc, in_=tmp8, axis=AX.X, op=ALU.add), reads=["rk"], writes=["rk"])
        for colv, rowv, c0 in ((hbc, hbRow, 256), (hcc, hcRow, 320)):
            ts("dve", dg32, identf[0:32, 0:32], colv, ALU.mult, ["rk", "cst"], ["rk"])
            mm(bank(5)[:, c0:c0 + 32], onesf[0:32, :], dg32, True, True, ["rk", "onesf"], [PSK(5)])
            cp("dve", rowv, bank(5)[:, c0:c0 + 32], [PSK(5)], ["hrows"])
        for t in range(NT):
            hb = h2tok[t % 2]
            xn2 = xn2s[t % 2]
            XN = ("xn2", t % 2)
            act(xn2, xres[:, t, :], AF.Identity, [("xres", t), "rstd_d"], [XN], scale=rstd_d[:, t:t + 1])
            tt("dve", h2tmp, xn2, rowS, ALU.mult, [XN, "row0"], ["h2tmp"])
            tt("pool", hb, h2tmp, rowB, ALU.add, ["h2tmp", "row1"], [("h2tok", t % 2)])
            ef = ef_all[:, t, :]
            oh = misc[:, 40:72]; oh2 = misc[:, 96:128]
            pk = misc[:, 72:76]; hbk = misc[:, 128:132]; hck = misc[:, 132:136]
            oh4 = misc2[:, 0:128].rearrange("p (k e) -> p k e", e=32)
            pr4 = misc2[:, 128:256].rearrange("p (k e) -> p k e", e=32)
            tt("dve", oh4, iota32.unsqueeze(1).broadcast_to([128, 4, 32]), ef.unsqueeze(2).broadcast_to([128, 4, 32]), ALU.is_equal,
               ["iota32", ("ef", t)], ["oh4"])
            for src, dst, sk_ in ((posf_all[:, t, :], pk, ("posf", t)), (hbRow, hbk, "hrows"), (hcRow, hck, "hrows")):
                tt("dve", pr4, oh4, src.unsqueeze(1).broadcast_to([128, 4, 32]), ALU.mult, ["oh4", sk_], ["pr4"])
                S.add("dve", lambda e, dst=dst, pr4=pr4: e.tensor_reduce(out=dst, in_=pr4, axis=AX.X, op=ALU.add),
                      reads=["pr4"], writes=["pkv"])
            instat = misc[:, 136:140]; fs = misc[:, 140:144]; q_ = misc[:, 144:148]; inh1 = misc[:, 148:152]; inh2 = misc[:, 152:156]
            fh = misc[:, 156:160]; flat = misc[:, 160:164]; valid = misc[:, 164:168]; ovf = misc[:, 168:172]
            V = ["pkv", "vec"]
            ts("dve", instat, pk, float(CAP), ALU.is_lt, V, ["vec"])
            stt(fs, ef, float(CAP), pk, ALU.mult, ALU.add, V + [("ef", t)], ["vec"])
            ts("dve", q_, pk, -float(CAP), ALU.add, V, ["vec"])
            ts("dve", inh1, q_, 0.0, ALU.is_ge, V, ["vec"])
            tt("dve", inh2, q_, hck, ALU.is_lt, V, ["vec"])
            tt("dve", inh1, inh1, inh2, ALU.mult, V, ["vec"])
            tt("dve", fh, hbk, q_, ALU.add, V, ["vec"])
            ts("dve", fh, fh, float(HOT0), ALU.add, V, ["vec"])
            tt("dve", fs, fs, instat, ALU.mult, V, ["vec"])
            tt("dve", fh, fh, inh1, ALU.mult, V, ["vec"])
            tt("dve", flat, fs, fh, ALU.add, V, ["vec"])
            tt("dve", valid, instat, inh1, ALU.add, V, ["vec"])
            ts("dve", ovf, valid, -1.0e6, ALU.mult, V, ["vec"], s2=1.0e6, op1=ALU.add)
            tt("dve", flat, flat, ovf, ALU.add, V, ["vec"])
            cp("dve", IDX[:, 4 * t:4 * t + 4], flat, V, [("IDX", t)])
            tt("dve", Gk[:, t, :], Gk[:, t, :], valid, ALU.mult, V + [("Gk", t)], [("Gk", t)])
            for k in range(4):
                S.add("pool", lambda e, t=t, k=k, hb=hb: e.indirect_dma_start(
                    out=Hs, out_offset=bass.IndirectOffsetOnAxis(ap=IDX[:, 4 * t + k:4 * t + k + 1], axis=0), in_=hb, in_offset=None,
                    bounds_check=bnd(e), oob_is_err=False),
                    reads=[("h2tok", t % 2), ("IDX", t)], writes=[("Hs", t, k)], semkey="hs_sc%d_%d" % (t % 2, k))
            tr(bank(6)[0:32, 0:128], Gt[:, t, :], identf, [("G", t), "cst"], [PSK(6)])
            cp("act", GTt[0:32, :], bank(6)[0:32, 0:128], [PSK(6)], ["GTt"])
            for nb in range(2):
                mm(bank(nb), GTt[0:32, :], bdp[0:32, nb * 512:(nb + 1) * 512], True, True, ["GTt", "bdp"], [PSK(nb)])
                tt("dve", xres[:, t, nb * 512:(nb + 1) * 512], xres[:, t, nb * 512:(nb + 1) * 512], bank(nb), ALU.add,
                   [PSK(nb), ("xres", t), XN], [("xres", t)])
        dump("d_x1", xres.rearrange("p t d -> p (t d)"), [("xres", t) for t in range(NT)])
        dump("d_G", Gt[:].rearrange("p t e -> p (t e)"), [("G", t) for t in range(NT)])
        dump("d_gk", Gk[:].rearrange("p t k -> p (t k)"), [("Gk", t) for t in range(NT)])

        S.mark('D')
        S.barrier()
        RE = R2
        MAXS = 1024
        hs_tok = [ab(RE + i * 8 * KB, 4 * 1024).rearrange("p (s d) -> p s d", d=1024) for i in range(2)]
        h2g = [ab(RE + 16 * KB + i * 16 * KB, 8 * MAXS).rearrange("p (k s) -> p k s", s=MAXS) for i in range(2)]
        actE = ab(RE + 48 * KB, 8 * MAXS).rearrange("p (k s) -> p k s", s=MAXS)
        slabE = [ab(RE + 64 * KB + i * 4 * KB, 8 * 256).rearrange("p (k c) -> p k c", c=256) for i in range(3)]
        wdE = [ab(RE + 76 * KB + i * 16 * KB, 8 * 1024).rearrange("p (k c) -> p k c", c=1024) for i in range(2)]
        ytile = [ab(RE + 108 * KB + i * 4 * KB, 1024) for i in range(2)]
        gcb = [af(RE + 116 * KB + i * 2 * KB, 512) for i in range(2)]
        u1b = [af(RE + 120 * KB + i * 2 * KB, 512) for i in range(2)]
        gsb = [ab(RE + 124 * KB + i * KB, 512) for i in range(2)]
        ykb = [ab(RE + i * 4 * KB, 1024) for i in range(12)]
        bu1 = prm[:, P_BU:P_BU + 256]
        ts("dve", bu1, bu1, 1.0, ALU.add, ["prm"], ["prm"])
        allHs = [("Hs", t, k) for t in range(NT) for k in range(4)]
        allYs = []
        cnt = {"sc": 0, "hl": 0, "tb": 0, "ew": 0, "yb": 0}
        g2row_ = grow[:, 1, :]
        ak = "actE"
        vals = {}

        def chunks(nt):
            out, t0 = [], 0
            while t0 < nt:
                n = min(4, nt - t0)
                out.append((t0, n))
                t0 += n
            return out

        def gather(pi_, row0, nt):
            hg = h2g[pi_ % 2]
            for (t0, n) in chunks(nt):
                hi = cnt["hl"] % 2
                cnt["hl"] += 1
                hb = hs_tok[hi]
                hk = ("hs_tok", hi)
                r0 = row0 + t0 * 128
                dma("sp", hb[:, 0:n, :], Hs[r0:r0 + n * 128, :].rearrange("(s p) d -> p s d", p=128), allHs, [hk], "hsld%d" % hi)
                for st_ in range(n):
                    bk = cnt["tb"] % 2
                    cnt["tb"] += 1
                    for kc in range(8):
                        tr(bankb(bk)[:, kc * 128:(kc + 1) * 128], hb[:, st_, kc * 128:(kc + 1) * 128], ident_b, [hk, "cbf"], [PSK(bk)])
                    c0 = (t0 + st_) * 128
                    cp("act", hg[:, :, c0:c0 + 128], bankb(bk)[:, 0:1024].rearrange("p (k s) -> p k s", s=128), [PSK(bk)], [("h2g", pi_ % 2)])

        def gate_up(pi_, nt, slab_dma, bgcol, bucol, bkeys):
            hg = h2g[pi_ % 2]
            gk_ = ("h2g", pi_ % 2)
            for j in range(8):
                sl = slabE[cnt["sc"] % 3]
                sk = ("slab", cnt["sc"] % 3)
                slab_dma(j, sl, sk, "slabld%d" % (cnt["sc"] % 3))
                cnt["sc"] += 1
                for (t0, n) in chunks(nt):
                    hsl = slice(t0 * 128, (t0 + n) * 128)
                    w_ = n * 128
                    i2 = cnt["ew"] % 2
                    cnt["ew"] += 1
                    pg, pu = 2 + i2 * 2, 3 + i2 * 2
                    for kc in range(8):
                        mm(bank(pg)[:, 0:w_], sl[:, kc, 0:128], hg[:, kc, hsl], kc == 0, kc == 7, [sk, gk_], [PSK(pg)])
                    for kc in range(8):
                        mm(bank(pu)[:, 0:w_], sl[:, kc, 128:256], hg[:, kc, hsl], kc == 0, kc == 7, [sk, gk_], [PSK(pu)])
                    ts("dve", gcb[i2][:, 0:w_], bank(pg)[:, 0:w_], bgcol(j), ALU.add, [PSK(pg)] + bkeys, [("gc", i2)], s2=7.0, op1=ALU.min)
                    act(gsb[i2][:, 0:w_], gcb[i2][:, 0:w_], AF.Gelu_apprx_sigmoid, [("gc", i2)], [("gs", i2)])
                    ts("dve", u1b[i2][:, 0:w_], bank(pu)[:, 0:w_], bucol(j), ALU.add, [PSK(pu)] + bkeys, [("u1", i2)], s2=8.0, op1=ALU.min)
                    stt(actE[:, j, hsl], u1b[i2][:, 0:w_], -6.0, gsb[i2][:, 0:w_], ALU.max, ALU.mult, [("u1", i2), ("gs", i2)], [ak])

        def down(pi_, row0, nt):
            wb = wdE[pi_ % 2]
            wk = ("wd", pi_ % 2)
            for st_ in range(nt):
                yb = cnt["yb"] % 2
                cnt["yb"] += 1
                yt_ = ytile[yb]
                for nb in range(2):
                    pd = 6 + nb
                    for kc in range(8):
                        mm(bank(pd), actE[:, kc, st_ * 128:(st_ + 1) * 128], wb[:, kc, nb * 512:(nb + 1) * 512], kc == 0, kc == 7,
                           [ak, wk], [PSK(pd)])
                    cp("act" if nb == 0 else "dve", yt_[:, nb * 512:(nb + 1) * 512], bank(pd), [PSK(pd)], [("ytile", yb)])
                r0 = row0 + st_ * 128
                dma("sp", Ys[r0:r0 + 128, :], yt_, [("ytile", yb)], [("Ys", r0)], "ys_st%d" % yb)
                allYs.append(("Ys", r0))

        def wd_scale(pi_):
            wb = wdE[pi_ % 2]
            wk = ("wd", pi_ % 2)
            for kc in range(8):
                tt("pool", wb[:, kc, :], wb[:, kc, :], g2row_, ALU.mult, [wk, ("grow", 1)], [wk])

        passes = [(e_ * CAP, CAP // 128, "s", e_) for e_ in range(nexp)]
        if nexp == NE:
            hb0 = 0
            for r_ in range(HOTN):
                passes.append((HOT0 + hb0 * 128, HOT_T[r_], "h", r_))
                hb0 += HOT_T[r_]

        def prep_pass(pi_):
            row0, nt, kind, id_ = passes[pi_]
            wb = wdE[pi_ % 2]
            wk = ("wd", pi_ % 2)
            if kind == "s":
                getrow = lambda id_=id_: wall_d[id_:id_ + 1, :]
                bgcol = lambda j, id_=id_: prm[:, P_BG + id_ * 8 + j:P_BG + id_ * 8 + j + 1]
                bucol = lambda j, id_=id_: bu1[:, id_ * 8 + j:id_ * 8 + j + 1]
                bkeys = ["prm"]
            else:
                r_ = id_

                def vload(e, r_=r_):
                    vals[r_] = e.value_load(pi_i[0:1, r_:r_ + 1])
                    return None
                S.add("pool", vload, reads=["pi_i"])
                getrow = lambda r_=r_: wall_d[bass.ds(vals[r_], 1), :]
                hbk_ = ("hbias", r_ % 2)
                S.add("pool", lambda e, r_=r_, getrow=getrow: e.dma_start(
                    out=hbias[:, r_ % 2, :], in_=getrow()[:, WOFF_B:WOFF_B + 2048].rearrange("e (p c) -> p (e c)", c=16)),
                    reads=[], writes=[hbk_], semkey="hbld%d" % (r_ % 2))
                ts("dve", hbias[:, r_ % 2, 8:16], hbias[:, r_ % 2, 8:16], 1.0, ALU.add, [hbk_], [hbk_])
                bgcol = lambda j, r_=r_: hbias[:, r_ % 2, j:j + 1]
                bucol = lambda j, r_=r_: hbias[:, r_ % 2, 8 + j:9 + j]
                bkeys = [hbk_]
            S.add("pool", lambda e, wb=wb, getrow=getrow: e.dma_start(
                out=wb, in_=getrow()[:, WOFF_D:WOFF_D + D * D].rearrange("e (k p c) -> p (e k) c", p=128, c=D)),
                reads=[], writes=[wk], semkey="wdld%d" % (pi_ % 2))

            def slab_dma(j, sl, sk, sem, getrow=getrow):
                S.add("pool", lambda e, j=j, sl=sl, getrow=getrow: e.dma_start(
                    out=sl, in_=getrow()[:, j * D * 256:(j + 1) * D * 256].rearrange("e (k p c) -> p (e k) c", p=128, c=256)),
                    reads=[], writes=[sk], semkey=sem)
            return (slab_dma, bgcol, bucol, bkeys)

        if passes:
            gather(0, passes[0][0], passes[0][1])
        for pi_ in range(len(passes)):
            row0, nt, kind, id_ = passes[pi_]
            src = prep_pass(pi_)
            gate_up(pi_, nt, *src)
            wd_scale(pi_)
            if pi_ + 1 < len(passes):
                gather(pi_ + 1, passes[pi_ + 1][0], passes[pi_ + 1][1])
            down(pi_, row0, nt)
        NYK = 12
        for b_ in range(NYK):
            memset("pool", ykb[b_], 0.0, [("yk", b_), ("hs_tok", b_ // 2) if b_ < 4 else ("h2g", (b_ - 4) // 4)])
        cnt_ = 0
        for t in range(NT):
            for k in range(4):
                b_ = cnt_ % NYK
                cnt_ += 1
                S.add("pool", lambda e, t=t, k=k, b_=b_: e.indirect_dma_start(
                    out=ykb[b_], out_offset=None, in_=Ys, in_offset=bass.IndirectOffsetOnAxis(ap=IDX[:, 4 * t + k:4 * t + k + 1], axis=0),
                    bounds_check=bnd(e), oob_is_err=False),
                    reads=allYs + [("IDX", t)], writes=[("yk", b_)], semkey="ykld%d" % b_)
                stt(xres[:, t, :], ykb[b_], Gk[:, t, k:k + 1], xres[:, t, :], ALU.mult, ALU.add,
                    [("yk", b_), ("Gk", t), ("xres", t)], [("xres", t)])
        S.disabled = False
        for t in range(NT):
            dma("sp", out_d[t * 128:(t + 1) * 128, :], xres[:, t, :], [("xres", t)], [("out", t)], "outst%d" % (t % 4))
        S.add("sp", None, reads=[("out", t) for t in range(NT)] + (["dbg_" + n for n in dbg] if debug else []))
        S.emit(nc, st)
    return nc


_CACHE = {}


def _prep(inp, b):
    f = lambda a: np.ascontiguousarray(a, dtype=np.float32)
    col = lambda v: f(np.asarray(v).reshape(-1, 128).T)
    prm = np.zeros((128, NPRM), np.float32)
    prm[:, P_CCOL:P_CCOL + 8] = col(inp["c"][b])
    prm[:, P_BADA:P_BADA + 48] = col(inp["b_ada"][0])
    prm[:, P_NMIX:P_NMIX + 8] = col(inp["norm_mix"][0])
    prm[:, P_NFFN:P_NFFN + 8] = col(inp["norm_ffn"][0])
    perm = []
    for i in range(4):
        perm += list(range(64 * i, 64 * i + 64)) + list(range(256 + 64 * i, 256 + 64 * i + 64))
    perm += list(range(512, 640)) + list(range(768, 1280)) + list(range(640, 768))
    perm = np.array(perm)
    b_in = inp["b_in"][0][perm]
    prm[:, P_BIN:P_BIN + 9] = col(b_in[:1152])
    prm[:, P_QG] = np.tile(inp["q_norm"][0], 2)
    prm[:, P_QG + 1] = np.tile(inp["k_norm"][0], 2)
    prm[:, P_LAMR:P_LAMR + 32] = np.tile(inp["lam_re"][0].T, (2, 1))
    prm[:, P_LAMI:P_LAMI + 32] = np.tile(inp["lam_im"][0].T, (2, 1))
    prm[:, P_LDT:P_LDT + 32] = np.tile(inp["log_dt"][0][None, :], (128, 1))
    prm[:, P_DSK:P_DSK + 4] = col(inp["d_skip"][0])
    prm[:, P_BGLU:P_BGLU + 4] = col(inp["b_glu"][0])
    prm[:, P_GCAT:P_GCAT + 8] = col(np.concatenate([inp["attn_out_norm"][0], inp["ssm_out_norm"][0]]))
    prm[:, P_SINK:P_SINK + 8] = np.tile(inp["sinks"][0][None, :], (128, 1))
    prm[:, P_BROUT:P_BROUT + 32] = np.tile(inp["b_router"][0][None, :], (128, 1))
    prm[:, P_BVROW:P_BVROW + 128] = np.tile(b_in[1152:1280][None, :], (128, 1))
    bgu = inp["b_gate_up"][0]
    prm[:, P_BG:P_BG + 256] = bgu[:, 0::2].reshape(32, 8, 128).transpose(2, 0, 1).reshape(128, 256)
    prm[:, P_BU:P_BU + 256] = bgu[:, 1::2].reshape(32, 8, 128).transpose(2, 0, 1).reshape(128, 256)
    return prm, perm


def _shared(inp):
    f = lambda a: np.ascontiguousarray(a, dtype=np.float32)
    perm = None
    _, perm = _prep(inp, 0)
    sh = {}
    sh["cst"] = _consts()
    sh["w_ada"] = f(inp["w_ada"][0])
    bada = inp["b_ada"][0]
    sh["bada_g"] = f(np.tile(np.concatenate([bada[2048:3072], bada[5120:6144]])[None, :], (128, 1)))
    sh["w_in"] = f(inp["w_in"][0][:, perm])
    bre = inp["b_re"][0].transpose(1, 0, 2).reshape(64, 512)
    bim = inp["b_im"][0].transpose(1, 0, 2).reshape(64, 512)
    sh["ssm_sb"] = f(np.stack([np.concatenate([bre, bim], 0), np.concatenate([bim, bre], 0)], axis=1))
    cre = inp["c_re"][0].transpose(2, 0, 1).reshape(64, 512)
    cim = inp["c_im"][0].transpose(2, 0, 1).reshape(64, 512)
    sh["ssm_sc"] = f(np.stack([np.concatenate([cre, cim], 0), np.concatenate([cim, cre], 0)], axis=1))
    sh["w_glu"] = f(inp["w_glu"][0])
    sh["w_out"] = f(inp["w_out"][0])
    sh["w_router"] = f(inp["w_router"][0])
    wgu = inp["w_gate_up"][0]
    g = wgu[:, :, 0::2].reshape(32, 1024, 8, 128)
    u = wgu[:, :, 1::2].reshape(32, 1024, 8, 128)
    w_gu = np.concatenate([g, u], axis=3).transpose(0, 2, 1, 3)
    bgu = inp["b_gate_up"][0]
    bg_ = bgu[:, 0::2].reshape(32, 8, 128).transpose(0, 2, 1)
    bu_ = bgu[:, 1::2].reshape(32, 8, 128).transpose(0, 2, 1)
    b_gu = np.concatenate([bg_, bu_], axis=2)
    sh["wall"] = f(np.concatenate([w_gu.reshape(32, -1), inp["w_down"][0].reshape(32, -1), b_gu.reshape(32, -1)], axis=1))
    sh["b_down"] = f(inp["b_down"][0])
    return sh


def kernel(**inputs):
    inp = {k: np.asarray(v) for k, v in inputs.items()}
    debug = bool(int(os.environ.get("MK_DEBUG", "0")))
    nexp = int(os.environ.get("MK_NEXP", str(NE)))
    cores = [int(c) for c in os.environ.get("MK_CORES", "0,1,2,3,4,5,6,7").split(",")]
    key = (debug, nexp, os.environ.get('MK_STOP', ''))
    if key not in _CACHE:
        _CACHE[key] = build(debug=debug, nexp=nexp)
    nc = _CACHE[key]
    sh = _shared(inp)
    in_maps = []
    for b in cores:
        m = dict(sh)
        m["x"] = np.ascontiguousarray(inp["x"][b], dtype=np.float32)
        m["prm"], _ = _prep(inp, b)
        in_maps.append(m)
    res = run_bass_kernel_spmd(nc, in_maps, core_ids=list(range(len(cores))))
    if debug:
        kernel.last = res.results
    out = np.zeros((8, S_, D), np.float32)
    for i, b in enumerate(cores):
        out[b] = res.results[i]["out"]
    return out
```
